# Optimizing a Trainium2 kernel written in Bass

```python
import jax, jax.numpy as jnp
from jax import lax
import numpy as np

D_MODEL = 1024
BATCH = 16
SEQ = 2048
DEPTH = 4

N_MIXERS = 2
N_HGRN_LAYERS = (DEPTH + 1) // 2
N_DSA_LAYERS = DEPTH // 2
N_DENSE_LAYERS = (DEPTH + 1) // 2
N_MOE_LAYERS = DEPTH // 2

HG_HEADS = 8
HG_DK = D_MODEL // HG_HEADS
HG_DV = D_MODEL // HG_HEADS
HG_CHUNK = 64

DSA_HEADS = 8
DSA_HEAD_DIM = 128
DSA_Q_RANK = 256
DSA_KV_RANK = 256
IDX_HEADS = 8
IDX_DIM = 64
IDX_TOPK_MAX = 256
Q_BLOCK = 128

D_FF = 3584
N_EXPERTS = 8
TOP_K_EXPERTS = 2

ALPHA = (2 * DEPTH) ** 0.25
BETA = (8 * DEPTH) ** -0.25
LN_EPS = 1e-5
RMS_EPS = 1e-6
NEG_BIG = -1e30

kernel_name = 'hgrn2_dsa_moe_deepnorm_hybrid'


def layer_norm(x, g, b):
    xf = x.astype(jnp.float32)
    mu = jnp.mean(xf, axis=-1, keepdims=True)
    var = jnp.mean(jnp.square(xf - mu), axis=-1, keepdims=True)
    return ((xf - mu) * lax.rsqrt(var + LN_EPS) * g + b).astype(x.dtype)


def rms_norm(x, g):
    xf = x.astype(jnp.float32)
    ms = jnp.mean(jnp.square(xf), axis=-1, keepdims=True)
    return (xf * lax.rsqrt(ms + RMS_EPS) * g).astype(x.dtype)


def swiglu(h, w_gate_up, w_down):
    gate, up = jnp.split(h @ w_gate_up, 2, axis=-1)
    return (jax.nn.silu(gate) * up) @ w_down


def hgrn2_mixer(x, w_in, lb, norm_g, w_out):
    B, S, _ = x.shape
    nc = S // HG_CHUNK
    q, f_pre, inp, g = jnp.split(x @ w_in, 4, axis=-1)
    f_pre = f_pre.astype(jnp.float32)
    f = lb + (1.0 - lb) * jax.nn.sigmoid(f_pre)
    log_f = jnp.log(f)
    k = (1.0 - lb) * jax.nn.sigmoid(-f_pre)

    def to_chunks(t, d):
        return t.astype(jnp.float32).reshape(B, nc, HG_CHUNK, HG_HEADS, d).transpose(1, 0, 3, 2, 4)

    qc, kc, lfc = to_chunks(q, HG_DK), to_chunks(k, HG_DK), to_chunks(log_f, HG_DK)
    vc = to_chunks(inp, HG_DV)
    causal = jnp.tril(jnp.ones((HG_CHUNK, HG_CHUNK), dtype=bool))[:, :, None]

    def step(state, chunk):
        q_, k_, v_, lf_ = chunk
        b = jnp.cumsum(lf_, axis=2)
        diff = b[:, :, :, None, :] - b[:, :, None, :, :]
        decay = jnp.where(causal, jnp.exp(jnp.where(causal, diff, 0.0)), 0.0)
        scores = jnp.einsum('bhtd,bhtsd,bhsd->bhts', q_, decay, k_)
        o = (jnp.einsum('bhts,bhsv->bhtv', scores, v_)
             + jnp.einsum('bhtd,bhdv->bhtv', q_ * jnp.exp(b), state))
        b_last = b[:, :, -1:, :]
        state = (jnp.exp(b_last[:, :, 0, :])[..., None] * state
                 + jnp.einsum('bhsd,bhsv->bhdv', k_ * jnp.exp(b_last - b), v_))
        return state, o

    state0 = jnp.zeros((B, HG_HEADS, HG_DK, HG_DV), jnp.float32)
    _, o = lax.scan(step, state0, (qc, kc, vc, lfc))
    o = o.transpose(1, 0, 3, 2, 4).reshape(B, S, HG_HEADS, HG_DV)
    o = rms_norm(o, norm_g) * jax.nn.silu(g.astype(jnp.float32)).reshape(B, S, HG_HEADS, HG_DV)
    return o.astype(x.dtype).reshape(B, S, D_MODEL) @ w_out


def dsa_mixer(x, w_in, q_norm_g, kv_norm_g, w_uq, w_uk, w_uv, w_qidx, kidx_norm_g, kidx_norm_b, w_out):
    B, S, _ = x.shape
    topk = min(IDX_TOPK_MAX, S // 4)
    splits = [DSA_Q_RANK, DSA_Q_RANK + DSA_KV_RANK, DSA_Q_RANK + DSA_KV_RANK + IDX_DIM]
    c_q, c_kv, k_idx, w_idx = jnp.split(x @ w_in, splits, axis=-1)
    c_q = rms_norm(c_q, q_norm_g)
    c_kv = rms_norm(c_kv, kv_norm_g)
    q = (c_q @ w_uq).reshape(B, S, DSA_HEADS, DSA_HEAD_DIM)
    q_lat = jnp.einsum('bshd,hdc->bshc', q, w_uk)
    q_idx = (c_q @ w_qidx).reshape(B, S, IDX_HEADS, IDX_DIM)
    k_idx = layer_norm(k_idx, kidx_norm_g, kidx_norm_b)
    w_idx = w_idx.astype(jnp.float32) * (IDX_HEADS ** -0.5 * IDX_DIM ** -0.5)
    key_pos = jnp.arange(S)

    def block(blk):
        t0 = blk * Q_BLOCK
        sl = lambda t: lax.dynamic_slice_in_dim(t, t0, Q_BLOCK, axis=1)
        qi, wi, ql = sl(q_idx), sl(w_idx), sl(q_lat)
        q_pos = t0 + jnp.arange(Q_BLOCK)
        rel = jax.nn.relu(jnp.einsum('bthd,bsd->bths', qi, k_idx).astype(jnp.float32))
        idx_score = jnp.einsum('bths,bth->bts', rel, wi)
        causal = key_pos[None, :] <= q_pos[:, None]
        idx_score = jnp.where(causal[None], idx_score, NEG_BIG)
        _, sel = lax.top_k(idx_score, topk)
        valid = sel <= q_pos[None, :, None]
        kv_sel = jax.vmap(lambda c, s: c[s])(c_kv, sel)
        logits = jnp.einsum('bthc,btkc->bthk', ql, kv_sel).astype(jnp.float32) * (DSA_HEAD_DIM ** -0.5)
        logits = jnp.where(valid[:, :, None, :], logits, NEG_BIG)
        p = jax.nn.softmax(logits, axis=-1).astype(kv_sel.dtype)
        return jnp.einsum('bthk,btkc->bthc', p, kv_sel)

    o_lat = lax.map(block, jnp.arange(S // Q_BLOCK))
    o_lat = jnp.moveaxis(o_lat, 0, 1).reshape(B, S, DSA_HEADS, DSA_KV_RANK)
    o = jnp.einsum('bshc,hcd->bshd', o_lat, w_uv).reshape(B, S, DSA_HEADS * DSA_HEAD_DIM)
    return o @ w_out


def moe_swiglu(h, w_router, w_gate_up, w_down):
    logits = (h @ w_router).astype(jnp.float32)
    top_vals, top_idx = lax.top_k(logits, TOP_K_EXPERTS)
    top_w = jax.nn.softmax(top_vals, axis=-1)
    gates = jnp.sum(jax.nn.one_hot(top_idx, N_EXPERTS, dtype=jnp.float32) * top_w[..., None], axis=-2)
    y = jnp.zeros_like(h)
    for e in range(N_EXPERTS):
        y = y + gates[..., e:e + 1].astype(h.dtype) * swiglu(h, w_gate_up[e], w_down[e])
    return y


def setup_inputs(seed: int = 0) -> dict:
    key = jax.random.key(seed)
    ks = iter(jax.random.split(key, 32))

    def w(shape, fan_in, scale=1.0):
        return jax.random.normal(next(ks), shape, jnp.float32) * (scale * fan_in ** -0.5)

    def near_one(shape):
        return 1.0 + 0.02 * jax.random.normal(next(ks), shape, jnp.float32)

    def small(shape, s=0.02):
        return s * jax.random.normal(next(ks), shape, jnp.float32)

    dsa_in_width = DSA_Q_RANK + DSA_KV_RANK + IDX_DIM + IDX_HEADS
    x = jax.random.normal(next(ks), (BATCH, SEQ, D_MODEL), jnp.float32)
    return {
        'x': x,
        'ln_g': near_one((DEPTH, 2, D_MODEL)),
        'ln_b': small((DEPTH, 2, D_MODEL)),
        'hg_w_in': w((N_HGRN_LAYERS, D_MODEL, 4 * D_MODEL), D_MODEL),
        'hg_lower_bounds': small((N_HGRN_LAYERS, D_MODEL), 0.5),
        'hg_norm_g': near_one((N_HGRN_LAYERS, HG_HEADS, HG_DV)),
        'hg_w_out': w((N_HGRN_LAYERS, D_MODEL, D_MODEL), D_MODEL, BETA),
        'dsa_w_in': w((N_DSA_LAYERS, D_MODEL, dsa_in_width), D_MODEL),
        'dsa_q_norm_g': near_one((N_DSA_LAYERS, DSA_Q_RANK)),
        'dsa_kv_norm_g': near_one((N_DSA_LAYERS, DSA_KV_RANK)),
        'dsa_w_uq': w((N_DSA_LAYERS, DSA_Q_RANK, DSA_HEADS * DSA_HEAD_DIM), DSA_Q_RANK),
        'dsa_w_uk': w((N_DSA_LAYERS, DSA_HEADS, DSA_HEAD_DIM, DSA_KV_RANK), DSA_HEAD_DIM),
        'dsa_w_uv': w((N_DSA_LAYERS, DSA_HEADS, DSA_KV_RANK, DSA_HEAD_DIM), DSA_KV_RANK),
        'dsa_w_qidx': w((N_DSA_LAYERS, DSA_Q_RANK, IDX_HEADS * IDX_DIM), DSA_Q_RANK),
        'dsa_kidx_norm_g': near_one((N_DSA_LAYERS, IDX_DIM)),
        'dsa_kidx_norm_b': small((N_DSA_LAYERS, IDX_DIM)),
        'dsa_w_out': w((N_DSA_LAYERS, DSA_HEADS * DSA_HEAD_DIM, D_MODEL), DSA_HEADS * DSA_HEAD_DIM, BETA),
        'ffn_w_gate_up': w((N_DENSE_LAYERS, D_MODEL, 2 * D_FF), D_MODEL),
        'ffn_w_down': w((N_DENSE_LAYERS, D_FF, D_MODEL), D_FF, BETA),
        'moe_w_router': w((N_MOE_LAYERS, D_MODEL, N_EXPERTS), D_MODEL),
        'moe_w_gate_up': w((N_MOE_LAYERS, N_EXPERTS, D_MODEL, 2 * D_FF), D_MODEL),
        'moe_w_down': w((N_MOE_LAYERS, N_EXPERTS, D_FF, D_MODEL), D_FF, BETA),
    }


def reference(x, ln_g, ln_b, hg_w_in, hg_lower_bounds, hg_norm_g, hg_w_out, dsa_w_in, dsa_q_norm_g,
              dsa_kv_norm_g, dsa_w_uq, dsa_w_uk, dsa_w_uv, dsa_w_qidx, dsa_kidx_norm_g, dsa_kidx_norm_b,
              dsa_w_out, ffn_w_gate_up, ffn_w_down, moe_w_router, moe_w_gate_up, moe_w_down):
    lb_all = jax.nn.softmax(hg_lower_bounds.astype(jnp.float32), axis=0)
    lb_all = jnp.cumsum(lb_all, axis=0) - lb_all[0]
    for layer in range(DEPTH):
        j = layer // N_MIXERS
        if layer % N_MIXERS == 0:
            mix = hgrn2_mixer(x, hg_w_in[j], lb_all[j], hg_norm_g[j], hg_w_out[j])
        else:
            mix = dsa_mixer(x, dsa_w_in[j], dsa_q_norm_g[j], dsa_kv_norm_g[j], dsa_w_uq[j], dsa_w_uk[j],
                            dsa_w_uv[j], dsa_w_qidx[j], dsa_kidx_norm_g[j], dsa_kidx_norm_b[j], dsa_w_out[j])
        x = layer_norm(ALPHA * x + mix, ln_g[layer, 0], ln_b[layer, 0])
        m = layer // 2
        if layer % 2 == 0:
            ff = swiglu(x, ffn_w_gate_up[m], ffn_w_down[m])
        else:
            ff = moe_swiglu(x, moe_w_router[m], moe_w_gate_up[m], moe_w_down[m])
        x = layer_norm(ALPHA * x + ff, ln_g[layer, 1], ln_b[layer, 1])
    return x
```

```python
from contextlib import ExitStack
import os
import numpy as np
import concourse.bass as bass
import concourse.mybir as mybir
from concourse.bass_utils import run_bass_kernel_spmd

F32 = mybir.dt.float32
BF16 = mybir.dt.bfloat16
AF = mybir.ActivationFunctionType
ALU = mybir.AluOpType
AX = mybir.AxisListType

D = 1024
SEQ = 2048
NSEQ = 2
T = NSEQ * SEQ
NT = T // 128
NTS = SEQ // 128
DEPTH = 4
DFF = 3584
NEXP = 8
ALPHA = (2 * DEPTH) ** 0.25
LN_EPS = 1e-5
RMS_EPS = 1e-6
NEG_BIG = -1e30
FC = 512
NFC = DFF // FC

NDMA = 40
NDMA_SP = 28


class Buf:
    __slots__ = ("name", "w", "r")

    def __init__(self, name=""):
        self.name = name
        self.w = None
        self.r = {}


class Sched:
    def __init__(self, nc, stack):
        self.nc = nc
        self.engs = {"pe": nc.tensor, "act": nc.scalar, "dve": nc.vector,
                     "pool": nc.gpsimd, "sp": nc.sync}
        self.sem = {k: stack.enter_context(nc.semaphore("s_" + k)) for k in self.engs}
        self.cnt = {k: 0 for k in self.engs}
        self.seen = {k: {o: 0 for o in self.engs} for k in self.engs}
        self.dsem = [stack.enter_context(nc.semaphore("d%d" % i)) for i in range(NDMA)]
        self.dcnt = [0] * NDMA
        self.dseen = {k: [0] * NDMA for k in self.engs}
        self.rr = 0
        self.rrs = {}
        self.ninst = 0

    def _wait(self, en, tok):
        eng = self.engs[en]
        if tok[0] == "e":
            _, e2, idx = tok
            if e2 == en and en == "pe":
                return
            if self.seen[en][e2] >= idx:
                return
            eng.wait_ge(self.sem[e2], idx)
            self.seen[en][e2] = idx
        else:
            _, j, val = tok
            if self.dseen[en][j] >= val:
                return
            eng.wait_ge(self.dsem[j], val)
            self.dseen[en][j] = val

    def _deps(self, en, reads, writes):
        for b in reads:
            if b.w is not None:
                self._wait(en, b.w)
        for b in writes:
            if b.w is not None:
                self._wait(en, b.w)
            for t in b.r.values():
                self._wait(en, t)

    def _commit(self, tok, reads, writes):
        key = tok[1] if tok[0] == "e" else ("d", tok[1])
        for b in reads:
            b.r[key] = tok
        for b in writes:
            b.w = tok
            b.r = {}

    def op(self, en, fn, reads=(), writes=()):
        self._deps(en, reads, writes)
        ins = fn(self.engs[en])
        self.cnt[en] += 1
        ins.then_inc(self.sem[en], 1)
        self.ninst += 1
        self._commit(("e", en, self.cnt[en]), reads, writes)

    def dma(self, en, out, in_, reads=(), writes=(), **kw):
        lo, hi = (0, NDMA_SP) if en == "sp" else (NDMA_SP, NDMA)
        j = self.rrs.get(en, lo)
        self.rrs[en] = lo + (j + 1 - lo) % (hi - lo)
        self._deps(en, reads, writes)
        if self.dcnt[j] > 0:
            self._wait(en, ("d", j, self.dcnt[j]))
        ins = self.engs[en].dma_start(out=out, in_=in_, **kw)
        self.dcnt[j] += 16
        ins.then_inc(self.dsem[j], 16)
        self.ninst += 1
        self._commit(("d", j, self.dcnt[j]), reads, writes)

    def barrier(self):
        for en in self.engs:
            for e2 in self.engs:
                if self.cnt[e2] > 0:
                    self._wait(en, ("e", e2, self.cnt[e2]))
            for j in range(NDMA):
                if self.dcnt[j] > 0:
                    self._wait(en, ("d", j, self.dcnt[j]))


class Ctx:
    pass


_UID = [0]


def uq(name):
    _UID[0] += 1
    return "%s_u%d" % (name, _UID[0])


def bufs(n, name=""):
    return [Buf("%s%d" % (name, i)) for i in range(n)]


class Epi:
    def __init__(self, S, nc, st, C, ln_g_row, ln_b_row, router=None, pt=None, b_pt=None, pr=None, b_pr=None, nbuf=2):
        self.S, self.nc, self.C = S, nc, C
        sb = lambda name, shape, dt: st.enter_context(nc.sbuf_tensor(uq(name), shape, dt))
        self.nbuf = nbuf
        self.xres = [sb("ep_xres%d" % i, [128, D], F32) for i in range(nbuf)]
        self.b_xres = bufs(nbuf, "xres")
        self.gbc = sb("ep_g", [128, D], F32)
        self.bbc = sb("ep_b", [128, D], F32)
        self.b_gb = Buf("gb")
        self.stats = sb("ep_stats", [128, 12], F32)
        self.mv = sb("ep_mv", [128, 8], F32)
        self.b_small = Buf("small")
        self.xT = [sb("ep_xT%d" % i, [128, 8, 128], BF16) for i in range(nbuf)]
        self.b_xT = bufs(nbuf, "xT")
        if pt is None:
            self.pt = [st.enter_context(nc.psum_tensor(uq("ep_pt%d" % i), [128, 4, 128], F32)) for i in range(2)]
            self.b_pt = bufs(2, "ept")
        else:
            self.pt, self.b_pt = pt, b_pt
        self.k = 0
        self.pk = 0
        S.dma("sp", self.gbc[:], ln_g_row.partition_broadcast(128), writes=[self.b_gb])
        S.dma("sp", self.bbc[:], ln_b_row.partition_broadcast(128), writes=[self.b_gb])
        self.router = router
        if router is not None:
            self.wr = sb("ep_wr", [128, 8, NEXP], F32)
            self.b_wr = Buf("wr")
            S.dma("sp", self.wr[:], router.rearrange("p (kc e) -> p kc e", e=NEXP), writes=[self.b_wr])
            self.xTf = sb("ep_xTf", [128, 8, 128], F32)
            self.b_xTf = Buf("xTf")
            if pr is None:
                self.pr = st.enter_context(nc.psum_tensor(uq("ep_pr"), [128, NEXP], F32))[:]
                self.b_pr = Buf("pr")
            else:
                self.pr, self.b_pr = pr, b_pr
            self.rt = sb("ep_rt", [128, 6, NEXP], F32)
            self.b_rt = Buf("rt")

    def prefetch(self, X_src, bX_src, tile):
        i = self.pk % self.nbuf
        self.pk += 1
        self.S.dma("sp", self.xres[i][:], X_src[tile * 128:(tile + 1) * 128, :],
                   reads=[bX_src[tile]], writes=[self.b_xres[i]])

    def run(self, tile, y_ap, y_bufs, X_dst, bX_dst, XT_dst, bXT_dst, gates_dst=None, bG=None):
        S, C = self.S, self.C
        i = self.k % self.nbuf
        self.k += 1
        xr, bx = self.xres[i], self.b_xres[i]
        st_, mv, bs = self.stats, self.mv, self.b_small
        if isinstance(y_ap, (list, tuple)):
            for hh, (yh, yb) in enumerate(zip(y_ap, y_bufs)):
                S.op("dve", lambda e: e.scalar_tensor_tensor(out=xr[:, hh * 512:(hh + 1) * 512],
                                                             in0=xr[:, hh * 512:(hh + 1) * 512], scalar=float(ALPHA),
                                                             in1=yh, op0=ALU.mult, op1=ALU.add),
                     reads=[yb, bx], writes=[bx])
        else:
            S.op("dve", lambda e: e.scalar_tensor_tensor(out=xr[:], in0=xr[:], scalar=float(ALPHA), in1=y_ap,
                                                         op0=ALU.mult, op1=ALU.add),
                 reads=list(y_bufs) + [bx], writes=[bx])
        S.op("dve", lambda e: e.bn_stats(out=st_[:, 0:6], in_=xr[:, 0:512]), reads=[bx], writes=[bs])
        S.op("dve", lambda e: e.bn_stats(out=st_[:, 6:12], in_=xr[:, 512:1024]), reads=[bx], writes=[bs])
        S.op("dve", lambda e: e.bn_aggr(out=mv[:, 0:2], in_=st_[:, 0:12]), reads=[bs], writes=[bs])
        S.op("dve", lambda e: e.tensor_scalar_add(out=mv[:, 2:3], in0=mv[:, 1:2], scalar1=float(LN_EPS)),
             reads=[bs], writes=[bs])
        S.op("act", lambda e: e.activation(out=mv[:, 3:4], in_=mv[:, 2:3], func=AF.Ln), reads=[bs], writes=[bs])
        S.op("act", lambda e: e.activation(out=mv[:, 4:5], in_=mv[:, 3:4], func=AF.Exp, scale=-0.5),
             reads=[bs], writes=[bs])
        S.op("dve", lambda e: e.tensor_scalar(out=mv[:, 5:6], in0=mv[:, 0:1], scalar1=-1.0, scalar2=mv[:, 4:5],
                                              op0=ALU.mult, op1=ALU.mult), reads=[bs], writes=[bs])
        S.op("act", lambda e: e.activation(out=xr[:], in_=xr[:], func=AF.Identity, scale=mv[:, 4:5],
                                           bias=mv[:, 5:6]), reads=[bs, bx], writes=[bx])
        S.op("pool", lambda e: e.tensor_tensor(out=xr[:], in0=xr[:], in1=self.gbc[:], op=ALU.mult),
             reads=[bx, self.b_gb], writes=[bx])
        S.op("pool", lambda e: e.tensor_tensor(out=xr[:], in0=xr[:], in1=self.bbc[:], op=ALU.add),
             reads=[bx, self.b_gb], writes=[bx])
        if X_dst is not None:
            S.dma("sp", X_dst[tile * 128:(tile + 1) * 128, :], xr[:], reads=[bx], writes=[bX_dst[tile]])
        if XT_dst is None:
            return
        xT, bxT = self.xT[i], self.b_xT[i]
        for half in range(2):
            pt, bpt = self.pt[half], self.b_pt[half]
            for q in range(4):
                kc = half * 4 + q
                S.op("pe", lambda e: e.transpose(pt[:, q, :], xr[:, kc * 128:(kc + 1) * 128], C.ident[:]),
                     reads=[bx, C.b_const], writes=[bpt])
            if self.router is not None:
                S.op("act", lambda e: e.activation(out=self.xTf[:, half * 4:(half + 1) * 4, :], in_=pt[:], func=AF.Copy),
                     reads=[bpt], writes=[self.b_xTf])
            S.op("act", lambda e: e.activation(out=xT[:, half * 4:(half + 1) * 4, :], in_=pt[:], func=AF.Copy),
                 reads=[bpt], writes=[bxT])
        S.dma("sp", XT_dst[tile], xT[:], reads=[bxT], writes=[bXT_dst[tile]])
        if self.router is not None:
            if os.environ.get("KDBG_RSTEP") == "0":
                return
            rt, brt = self.rt, self.b_rt
            for kc in range(8):
                S.op("pe", lambda e: e.matmul(self.pr, lhsT=self.xTf[:, kc, :], rhs=self.wr[:, kc, :],
                                              start=(kc == 0), stop=(kc == 7)),
                     reads=[self.b_xTf, self.b_wr], writes=[self.b_pr])
            S.op("dve", lambda e: e.tensor_copy(out=rt[:, 0, :], in_=self.pr), reads=[self.b_pr], writes=[brt])
            if os.environ.get("KDBG_RSTEP") == "1":
                return
            S.op("dve", lambda e: e.max(out=rt[:, 1, :], in_=rt[:, 0, :]), reads=[brt], writes=[brt])
            S.op("dve", lambda e: e.tensor_scalar_mul(out=rt[:, 2, 0:1], in0=rt[:, 1, 0:1], scalar1=-1.0),
                 reads=[brt], writes=[brt])
            S.op("act", lambda e: e.activation(out=rt[:, 3, :], in_=rt[:, 0, :], func=AF.Exp, bias=rt[:, 2, 0:1],
                                               scale=1.0), reads=[brt], writes=[brt])
            S.op("dve", lambda e: e.scalar_tensor_tensor(out=rt[:, 4, :], in0=rt[:, 0, :], scalar=rt[:, 1, 1:2],
                                                         in1=rt[:, 3, :], op0=ALU.is_ge, op1=ALU.mult),
                 reads=[brt], writes=[brt])
            S.op("dve", lambda e: e.reduce_sum(out=rt[:, 2, 1:2], in_=rt[:, 4, :], axis=AX.X),
                 reads=[brt], writes=[brt])
            S.op("dve", lambda e: e.reciprocal(out=rt[:, 2, 2:3], in_=rt[:, 2, 1:2]), reads=[brt], writes=[brt])
            S.op("dve", lambda e: e.tensor_scalar_mul(out=rt[:, 5, :], in0=rt[:, 4, :], scalar1=rt[:, 2, 2:3]),
                 reads=[brt], writes=[brt])
            S.dma("sp", gates_dst[tile * 128:(tile + 1) * 128, :], rt[:, 5, :], reads=[brt], writes=[bG[tile]])


def phase_prep(S, nc, C, X_src, bX_src, XT_dst, bXT_dst):
    with ExitStack() as st:
        sb = lambda name, shape, dt: st.enter_context(nc.sbuf_tensor(uq(name), shape, dt))
        xin = [sb("pp_x%d" % i, [128, D], F32) for i in range(2)]
        b_xin = bufs(2)
        xT = [sb("pp_xT%d" % i, [128, 8, 128], BF16) for i in range(2)]
        b_xT = bufs(2)
        pt = [st.enter_context(nc.psum_tensor(uq("pp_pt%d" % i), [128, 4, 128], F32)) for i in range(2)]
        b_pt = bufs(2)
        for tile in range(NT):
            i = tile % 2
            S.dma("sp", xin[i][:], X_src[tile * 128:(tile + 1) * 128, :], reads=[bX_src[tile]], writes=[b_xin[i]])
            for half in range(2):
                for q in range(4):
                    kc = half * 4 + q
                    S.op("pe", lambda e: e.transpose(pt[half][:, q, :], xin[i][:, kc * 128:(kc + 1) * 128],
                                                     C.ident[:]),
                         reads=[b_xin[i], C.b_const], writes=[b_pt[half]])
                eng = "act" if half == 0 else "dve"
                if eng == "act":
                    S.op("act", lambda e: e.activation(out=xT[i][:, half * 4:(half + 1) * 4, :], in_=pt[half][:],
                                                       func=AF.Copy), reads=[b_pt[half]], writes=[b_xT[i]])
                else:
                    S.op("dve", lambda e: e.tensor_copy(out=xT[i][:, half * 4:(half + 1) * 4, :], in_=pt[half][:]),
                         reads=[b_pt[half]], writes=[b_xT[i]])
            S.dma("sp", XT_dst[tile], xT[i][:], reads=[b_xT[i]], writes=[bXT_dst[tile]])
        S.barrier()


def phase_ffn(S, nc, C, XT_src, bXT_src, X_res, bX_res, X_dst, bX_dst, XT_dst, bXT_dst,
              experts, gates, bG, ln_g_row, ln_b_row):
    moe = gates is not None
    with ExitStack() as st:
        sb = lambda name, shape, dt: st.enter_context(nc.sbuf_tensor(uq(name), shape, dt))
        yacc = sb("ff_y", [128, NTS, D], F32)
        b_y = bufs(NTS, "y")
        xt = sb("ff_xt", [128, NTS, 8, 128], BF16)
        b_xt = bufs(4, "xt")
        NW = 2
        wgu = [sb("ff_wgu%d" % i, [128, 2, 8, FC], BF16) for i in range(NW)]
        wg = [w_[:, 0, :, :] for w_ in wgu]
        wu = [w_[:, 1, :, :] for w_ in wgu]
        wd = [sb("ff_wd%d" % i, [128, FC // 128, D], BF16) for i in range(NW)]
        b_w = bufs(NW, "w")
        NFS = FC // 128
        hT = [sb("ff_h%d" % i, [128, NFS, 512], BF16) for i in range(2)]
        b_h = bufs(2, "h")
        sg = [sb("ff_s%d" % i, [128, 512], BF16) for i in range(2)]
        b_s = bufs(2, "s")
        if moe:
            gt = sb("ff_gt", [128, NTS, NEXP], F32)
            b_gt = Buf("gt")
        pg = [st.enter_context(nc.psum_tensor(uq("ff_pg%d" % i), [128, 512], F32)) for i in range(2)]
        pu = [st.enter_context(nc.psum_tensor(uq("ff_pu%d" % i), [128, 512], F32)) for i in range(2)]
        b_pg, b_pu = bufs(2, "pg"), bufs(2, "pu")
        py = [st.enter_context(nc.psum_tensor(uq("ff_py%d" % i), [128, 512], F32)) for i in range(2)]
        b_py = bufs(2, "py")
        epi = Epi(S, nc, st, C, ln_g_row, ln_b_row)

        for seq in range(NSEQ):
            t0 = seq * SEQ
            for blk in range(4):
                S.dma("sp", xt[:, blk * 4:(blk + 1) * 4, :, :],
                      XT_src[t0 // 128 + blk * 4:t0 // 128 + (blk + 1) * 4].rearrange("n p kc t -> p n kc t"),
                      reads=[bXT_src[(t0 + blk * 512) // 128 + q] for q in range(4)], writes=[b_xt[blk]])
            if moe:
                for q in range(NTS):
                    S.dma("sp", gt[:, q, :], gates[t0 + q * 128:t0 + (q + 1) * 128, :],
                          reads=[bG[t0 // 128 + q]], writes=[b_gt])
            chunks = [(e, fc) for e in range(len(experts)) for fc in range(NFC)]
            items = [(ci, tb) for ci in range(len(chunks)) for tb in range(4)]

            def load_w(ci):
                e, fc = chunks[ci]
                w_gu, w_d = experts[e]
                sl = ci % NW
                S.dma("pool", wgu[sl][:].rearrange("p a k f -> p (a k f)").rearrange("p (c f) -> p c f", f=2048),
                      w_gu[fc].rearrange("p (c f) -> p c f", f=2048), writes=[b_w[sl]])
                S.dma("pool", wd[sl][:].rearrange("p a n -> p (a n)").rearrange("p (c f) -> p c f", f=2048),
                      w_d[fc].rearrange("p (c f) -> p c f", f=2048), writes=[b_w[sl]])

            def up(k):
                ci, tb = items[k]
                sl = ci % NW
                h, bh = hT[k % 2], b_h[k % 2]
                for fs in range(NFS):
                    j = (k * NFS + fs) % 2
                    for kc in range(8):
                        S.op("pe", lambda e: e.matmul(pg[j][:], lhsT=wg[sl][:, kc, fs * 128:(fs + 1) * 128],
                                                      rhs=xt[:, tb * 4:(tb + 1) * 4, kc, :],
                                                      start=(kc == 0), stop=(kc == 7)),
                             reads=[b_w[sl], b_xt[tb]], writes=[b_pg[j]])
                    for kc in range(8):
                        S.op("pe", lambda e: e.matmul(pu[j][:], lhsT=wu[sl][:, kc, fs * 128:(fs + 1) * 128],
                                                      rhs=xt[:, tb * 4:(tb + 1) * 4, kc, :],
                                                      start=(kc == 0), stop=(kc == 7)),
                             reads=[b_w[sl], b_xt[tb]], writes=[b_pu[j]])
                    S.op("act", lambda e: e.activation(out=sg[j][:], in_=pg[j][:], func=AF.Silu),
                         reads=[b_pg[j]], writes=[b_s[j]])
                    S.op("dve", lambda e: e.tensor_tensor(out=h[:, fs, :], in0=sg[j][:], in1=pu[j][:], op=ALU.mult),
                         reads=[b_s[j], b_pu[j]], writes=[bh])

            def down(k):
                ci, tb = items[k]
                e_idx, fc = chunks[ci]
                sl = ci % NW
                h, bh = hT[k % 2], b_h[k % 2]
                for ts in range(4):
                    tl = tb * 4 + ts
                    for nh in range(2):
                        j = (ts * 2 + nh) % 2
                        for fs in range(NFS):
                            S.op("pe", lambda e: e.matmul(py[j][:], lhsT=h[:, fs, ts * 128:(ts + 1) * 128],
                                                          rhs=wd[sl][:, fs, nh * 512:(nh + 1) * 512],
                                                          start=(fs == 0), stop=(fs == NFS - 1)),
                                 reads=[bh, b_w[sl]], writes=[b_py[j]])
                        ya = yacc[:, tl, nh * 512:(nh + 1) * 512]
                        if moe:
                            gsc = gt[:, tl, e_idx:e_idx + 1]
                            if ci == 0:
                                S.op("dve", lambda e: e.tensor_scalar_mul(out=ya, in0=py[j][:], scalar1=gsc),
                                     reads=[b_py[j], b_gt], writes=[b_y[tl]])
                            else:
                                S.op("dve", lambda e: e.scalar_tensor_tensor(out=ya, in0=py[j][:], scalar=gsc, in1=ya,
                                                                             op0=ALU.mult, op1=ALU.add),
                                     reads=[b_py[j], b_gt, b_y[tl]], writes=[b_y[tl]])
                        else:
                            if ci == 0:
                                S.op("dve", lambda e: e.tensor_copy(out=ya, in_=py[j][:]),
                                     reads=[b_py[j]], writes=[b_y[tl]])
                            else:
                                S.op("dve", lambda e: e.tensor_tensor(out=ya, in0=py[j][:], in1=ya, op=ALU.add),
                                     reads=[b_py[j], b_y[tl]], writes=[b_y[tl]])

            load_w(0)
            for k in range(len(items)):
                ci, tb = items[k]
                if tb == 0 and ci + 1 < len(chunks):
                    load_w(ci + 1)
                if k == 0:
                    up(0)
                if k + 1 < len(items):
                    up(k + 1)
                down(k)
            epi.prefetch(X_res, bX_res, t0 // 128)
            for tl in range(NTS):
                tile = t0 // 128 + tl
                if tl + 1 < NTS:
                    epi.prefetch(X_res, bX_res, tile + 1)
                epi.run(tile, yacc[:, tl, :], [b_y[tl]], X_dst, bX_dst, XT_dst, bXT_dst)
        S.barrier()


def phase_hgrn(S, nc, C, j, W, XT_src, bXT_src, X_res, bX_res, X_dst, bX_dst, XT_dst, bXT_dst,
               ln_g_row, ln_b_row):
    w_in_d, w_out_d = W["hg_w_in"][j], W["hg_w_out"][j]
    with ExitStack() as st:
        sb = lambda name, shape, dt: st.enter_context(nc.sbuf_tensor(uq(name), shape, dt))
        ps = lambda name, shape, dt: st.enter_context(nc.psum_tensor(uq(name), shape, dt))
        w_in = sb("hg_win", [128, 8, 4096], BF16)
        w_out = sb("hg_wout", [128, 8, D], BF16)
        b_w = Buf("hgw")
        for sec in range(4):
            S.dma("pool", w_in[:, :, sec * 1024:(sec + 1) * 1024],
                  w_in_d[:, sec * 1024:(sec + 1) * 1024].rearrange("(kc p) n -> p kc n", p=128), writes=[b_w])
        S.dma("pool", w_out[:], w_out_d.rearrange("(kc p) n -> p kc n", p=128), writes=[b_w])
        sc = sb("hg_sc", [128, 5, 8], F32)
        b_sc = Buf("hgsc")
        S.dma("sp", sc[:, 4, :], W["hg_norm_g"][j].rearrange("h p -> p h"), writes=[b_sc],
              allow_slow_non_contiguous=True)
        if j == 0:
            S.op("dve", lambda e: e.memset(sc[:, 2, :], 0.0), writes=[b_sc])
            S.op("dve", lambda e: e.memset(sc[:, 3, :], 1.0), writes=[b_sc])
        else:
            S.dma("sp", sc[:, 0, :], W["hg_lower_bounds"][0].rearrange("(h p) -> p h", p=128), writes=[b_sc],
                  allow_slow_non_contiguous=True)
            S.dma("sp", sc[:, 1, :], W["hg_lower_bounds"][1].rearrange("(h p) -> p h", p=128), writes=[b_sc],
                  allow_slow_non_contiguous=True)
            S.op("dve", lambda e: e.tensor_tensor(out=sc[:, 1, :], in0=sc[:, 1, :], in1=sc[:, 0, :], op=ALU.subtract),
                 reads=[b_sc], writes=[b_sc])
            S.op("act", lambda e: e.activation(out=sc[:, 2, :], in_=sc[:, 1, :], func=AF.Sigmoid),
                 reads=[b_sc], writes=[b_sc])
            S.op("dve", lambda e: e.tensor_scalar(out=sc[:, 3, :], in0=sc[:, 2, :], scalar1=-1.0, scalar2=1.0,
                                                  op0=ALU.mult, op1=ALU.add), reads=[b_sc], writes=[b_sc])
        rmask = sb("hg_rmask", [128, 8, 64], F32)
        cmask = sb("hg_cmask", [64, 64], F32)
        ones = sb("hg_ones", [128, 128], F32)
        epsb = sb("hg_eps", [128, 1], F32)
        b_k = Buf("hgconst")
        S.op("pool", lambda e: e.memset(rmask[:], 1.0), writes=[b_k])
        S.op("pool", lambda e: e.memset(rmask[:, :, 0:1], 0.0), reads=[b_k], writes=[b_k])
        S.op("pool", lambda e: e.memset(cmask[:], 1.0), reads=[b_k], writes=[b_k])
        S.op("pool", lambda e: e.affine_select(out=cmask[:], in_=cmask[:], pattern=[[1, 64]], compare_op=ALU.is_ge,
                                               fill=0.0, base=0, channel_multiplier=-1), reads=[b_k], writes=[b_k])
        S.op("pool", lambda e: e.memset(ones[:], 1.0), reads=[b_k], writes=[b_k])
        S.op("pool", lambda e: e.memset(epsb[:], float(RMS_EPS)), reads=[b_k], writes=[b_k])
        xt = [sb("hg_xt%d" % i, [128, 4, 8, 128], BF16) for i in range(2)]
        b_xt = bufs(2, "hgxt")
        f_ = sb("hg_f", [128, 512], F32)
        lf = sb("hg_lf", [128, 512], F32)
        bb = sb("hg_b", [128, 512], F32)
        eb = sb("hg_eb", [128, 512], F32)
        enb = sb("hg_enb", [128, 512], F32)
        b_f, b_lf, b_bb, b_eb, b_enb = [Buf(n) for n in ("f", "lf", "bb", "eb", "enb")]
        qn = sb("hg_qn", [128, 512], BF16)
        kn = sb("hg_kn", [128, 512], BF16)
        sgt = sb("hg_sgt", [128, 512], BF16)
        b_qn, b_kn, b_sgt = Buf("qn"), Buf("kn"), Buf("sgt")
        v_sb = sb("hg_v", [64, 8, 128], BF16)
        kn_tok = sb("hg_kntok", [64, 8, 128], BF16)
        b_v, b_kntok = Buf("v"), Buf("kntok")
        scm = [sb("hg_scm%d" % i, [64, 64], BF16) for i in range(2)]
        b_scm = bufs(2, "scm")
        Sf = sb("hg_Sf", [128, 8, 128], F32)
        Sbf = sb("hg_Sbf", [128, 8, 128], BF16)
        b_Sf, b_Sbf = bufs(8, "Sf"), bufs(8, "Sbf")
        sq = sb("hg_sq", [128, 512], F32)
        rstd = sb("hg_rstd", [128, 512], F32)
        on = sb("hg_on", [128, 512], F32)
        b_sq, b_rstd, b_on = Buf("sq"), Buf("rstd"), Buf("on")
        onT = sb("hg_onT", [128, 8, 512], BF16)
        b_onT = bufs(8, "onT")
        ymix = sb("hg_ymix", [128, D], F32)
        b_ymix = Buf("ymix")
        pq, pf, pg = ps("hg_pq", [128, 512], F32), ps("hg_pf", [128, 512], F32), ps("hg_pg", [128, 512], F32)
        b_pq, b_pf, b_pg = Buf("pq"), Buf("pf"), Buf("pg")
        pv = [ps("hg_pv%d" % i, [128, 4, 128], F32) for i in range(2)]
        b_pv = bufs(2, "pv")
        po = ps("hg_po", [128, 512], F32)
        b_po = Buf("po")
        pmisc = ps("hg_pmisc", [128, 512], F32)
        b_psc, b_pkv = bufs(2, "psc"), Buf("pkv")
        ptr = ps("hg_ptr", [64, 8, 128], BF16)
        b_ptr = Buf("ptr")
        epi = Epi(S, nc, st, C, ln_g_row, ln_b_row, pt=pv, b_pt=b_pv)

        def load_xt(gb):
            i = gb % 2
            S.dma("sp", xt[i][:], XT_src[gb * 4:(gb + 1) * 4].rearrange("n p kc t -> p n kc t"),
                  reads=[bXT_src[gb * 4 + q] for q in range(4)], writes=[b_xt[i]])

        load_xt(0)
        for gb in range(T // 512):
            seq_start = (gb % (SEQ // 512)) == 0
            if gb + 1 < T // 512:
                load_xt(gb + 1)
            x, bx = xt[gb % 2], b_xt[gb % 2]
            for h in range(8):
                hs = slice(h * 128, (h + 1) * 128)
                for (p, bp, off) in ((pq, b_pq, 0), (pf, b_pf, 1024), (pg, b_pg, 3072)):
                    for kc in range(8):
                        S.op("pe", lambda e: e.matmul(p[:], lhsT=w_in[:, kc, off + h * 128:off + (h + 1) * 128],
                                                      rhs=x[:, :, kc, :], start=(kc == 0), stop=(kc == 7)),
                             reads=[b_w, bx], writes=[bp])
                for c in range(8):
                    for kc in range(8):
                        S.op("pe", lambda e: e.matmul(pv[c // 4][0:64, c % 4, :], lhsT=x[:, c // 2, kc, (c % 2) * 64:(c % 2) * 64 + 64],
                                                      rhs=w_in[:, kc, 2048 + h * 128:2048 + (h + 1) * 128],
                                                      start=(kc == 0), stop=(kc == 7)),
                             reads=[b_w, bx], writes=[b_pv[c // 4]])
                S.op("act", lambda e: e.activation(out=f_[:], in_=pf[:], func=AF.Sigmoid), reads=[b_pf], writes=[b_f])
                S.op("dve", lambda e: e.tensor_scalar(out=f_[:], in0=f_[:], scalar1=sc[:, 3, h:h + 1],
                                                      scalar2=sc[:, 2, h:h + 1], op0=ALU.mult, op1=ALU.add),
                     reads=[b_f, b_sc], writes=[b_f])
                S.op("act", lambda e: e.activation(out=lf[:], in_=f_[:], func=AF.Ln), reads=[b_f], writes=[b_lf])
                S.op("dve", lambda e: e.tensor_tensor_scan(out=bb[:], data0=rmask[:].rearrange("p a b -> p (a b)"),
                                                           data1=lf[:], initial=0.0, op0=ALU.mult, op1=ALU.add),
                     reads=[b_lf, b_k], writes=[b_bb])
                S.op("act", lambda e: e.activation(out=eb[:], in_=bb[:], func=AF.Exp), reads=[b_bb], writes=[b_eb])
                S.op("act", lambda e: e.activation(out=enb[:], in_=bb[:], func=AF.Exp, scale=-1.0),
                     reads=[b_bb], writes=[b_enb])
                S.op("dve", lambda e: e.scalar_tensor_tensor(out=qn[:], in0=pq[:], scalar=-1.0, in1=eb[:],
                                                             op0=ALU.mult, op1=ALU.mult),
                     reads=[b_pq, b_eb], writes=[b_qn])
                S.op("dve", lambda e: e.scalar_tensor_tensor(out=kn[:], in0=f_[:], scalar=1.0, in1=enb[:],
                                                             op0=ALU.subtract, op1=ALU.mult),
                     reads=[b_f, b_enb], writes=[b_kn])
                S.op("act", lambda e: e.activation(out=sgt[:], in_=pg[:], func=AF.Silu), reads=[b_pg], writes=[b_sgt])
                for half in range(2):
                    S.op("act", lambda e: e.activation(out=v_sb[:, half * 4:(half + 1) * 4, :],
                                                       in_=pv[half][0:64, :, :], func=AF.Copy),
                         reads=[b_pv[half]], writes=[b_v])
                for c in range(8):
                    S.op("pe", lambda e: e.transpose(ptr[:, c, :], kn[:, c * 64:(c + 1) * 64], C.identb[:]),
                         reads=[b_kn, C.b_const], writes=[b_ptr])
                S.op("dve", lambda e: e.tensor_copy(out=kn_tok[:], in_=ptr[:]), reads=[b_ptr], writes=[b_kntok])
                for c in range(8):
                    cs = slice(c * 64, (c + 1) * 64)
                    k2 = c % 2
                    psc = pmisc[0:64, k2 * 64:(k2 + 1) * 64]
                    pkv = pmisc[:, 128:256]
                    first = seq_start and c == 0
                    S.op("pe", lambda e: e.matmul(psc, lhsT=kn[:, cs], rhs=qn[:, cs], start=True, stop=True),
                         reads=[b_kn, b_qn], writes=[b_psc[k2]])
                    S.op("dve", lambda e: e.tensor_tensor(out=scm[k2][:], in0=psc, in1=cmask[:], op=ALU.mult),
                         reads=[b_psc[k2], b_k], writes=[b_scm[k2]])
                    S.op("pe", lambda e: e.matmul(po[:, cs], lhsT=v_sb[:, c, :], rhs=scm[k2][:], start=True, stop=first),
                         reads=[b_v, b_scm[k2]], writes=[b_po])
                    if not first:
                        S.op("pe", lambda e: e.matmul(po[:, cs], lhsT=Sbf[:, h, :], rhs=qn[:, cs], start=False, stop=True),
                             reads=[b_Sbf[h], b_qn], writes=[b_po])
                    S.op("pe", lambda e: e.matmul(pkv, lhsT=kn_tok[:, c, :], rhs=v_sb[:, c, :], start=True, stop=True),
                         reads=[b_kntok, b_v], writes=[b_pkv])
                    ebl = eb[:, c * 64 + 63:c * 64 + 64]
                    if first:
                        S.op("dve", lambda e: e.tensor_scalar_mul(out=Sf[:, h, :], in0=pkv, scalar1=ebl),
                             reads=[b_pkv, b_eb], writes=[b_Sf[h]])
                    else:
                        S.op("dve", lambda e: e.tensor_scalar_mul(out=Sf[:, h, :], in0=Sf[:, h, :], scalar1=ebl),
                             reads=[b_eb, b_Sf[h]], writes=[b_Sf[h]])
                        S.op("dve", lambda e: e.scalar_tensor_tensor(out=Sf[:, h, :], in0=pkv, scalar=ebl,
                                                                     in1=Sf[:, h, :], op0=ALU.mult, op1=ALU.add),
                             reads=[b_pkv, b_eb, b_Sf[h]], writes=[b_Sf[h]])
                    S.op("act", lambda e: e.activation(out=Sbf[:, h, :], in_=Sf[:, h, :], func=AF.Copy),
                         reads=[b_Sf[h]], writes=[b_Sbf[h]])
                S.op("act", lambda e: e.activation(out=sq[:], in_=po[:], func=AF.Square), reads=[b_po], writes=[b_sq])
                S.op("pe", lambda e: e.matmul(pq[:], lhsT=ones[:], rhs=sq[:], start=True, stop=True),
                     reads=[b_sq, b_k], writes=[b_pq])
                S.op("act", lambda e: e.activation(out=rstd[:], in_=pq[:], func=AF.Ln, scale=1.0 / 128.0,
                                                   bias=epsb[:, 0:1]), reads=[b_pq, b_k], writes=[b_rstd])
                S.op("act", lambda e: e.activation(out=rstd[:], in_=rstd[:], func=AF.Exp, scale=-0.5),
                     reads=[b_rstd], writes=[b_rstd])
                S.op("dve", lambda e: e.tensor_tensor(out=on[:], in0=po[:], in1=rstd[:], op=ALU.mult),
                     reads=[b_po, b_rstd], writes=[b_on])
                S.op("dve", lambda e: e.scalar_tensor_tensor(out=onT[:, h, :], in0=on[:], scalar=sc[:, 4, h:h + 1],
                                                             in1=sgt[:], op0=ALU.mult, op1=ALU.mult),
                     reads=[b_on, b_sgt, b_sc], writes=[b_onT[h]])
            epi.prefetch(X_res, bX_res, gb * 4)
            for ts in range(4):
                tile = gb * 4 + ts
                if ts + 1 < 4:
                    epi.prefetch(X_res, bX_res, tile + 1)
                for nh, (p, bp) in enumerate(((pf, b_pf), (pg, b_pg))):
                    for h in range(8):
                        S.op("pe", lambda e: e.matmul(p[:], lhsT=onT[:, h, ts * 128:(ts + 1) * 128],
                                                      rhs=w_out[:, h, nh * 512:(nh + 1) * 512],
                                                      start=(h == 0), stop=(h == 7)),
                             reads=[b_onT[h], b_w], writes=[bp])
                    S.op("act", lambda e: e.activation(out=ymix[:, nh * 512:(nh + 1) * 512], in_=p[:], func=AF.Copy),
                         reads=[bp], writes=[b_ymix])
                epi.run(tile, ymix[:], [b_ymix], X_dst, bX_dst, XT_dst, bXT_dst)
        S.barrier()


TOPK = 256
IDX_SCALE = (8 ** -0.5) * (64 ** -0.5)
ATT_SCALE = 128 ** -0.5


def phase_dsa(S, nc, C, j, W, XT_src, bXT_src, X_res, bX_res, X_dst, bX_dst, XT_dst, bXT_dst,
              ln_g_row, ln_b_row, router, Gt, bG):
    with ExitStack() as st:
        sb = lambda name, shape, dt: st.enter_context(nc.sbuf_tensor(uq(name), shape, dt))
        ps = lambda name, shape, dt: st.enter_context(nc.psum_tensor(uq(name), shape, dt))
        win = sb("ds_win", [128, 8, 584], BF16)
        wuq = sb("ds_wuq", [128, 2, 1024], BF16)
        wqi = sb("ds_wqi", [128, 2, 512], BF16)
        wuk = sb("ds_wuk", [128, 8, 256], BF16)
        wuv = sb("ds_wuv", [128, 8, 2, 128], BF16)
        wout = sb("ds_wout", [128, 8, D], BF16)
        b_w = Buf("dsw")
        S.dma("pool", win[:], W["dsa_w_in"][j].rearrange("(kc p) n -> p kc n", p=128), writes=[b_w])
        S.dma("pool", wuq[:], W["dsa_w_uq"][j].rearrange("(kc p) n -> p kc n", p=128), writes=[b_w])
        S.dma("pool", wqi[:], W["dsa_w_qidx"][j].rearrange("(kc p) n -> p kc n", p=128), writes=[b_w])
        S.dma("pool", wuk[:], W["dsa_w_uk"][j].rearrange("h d c -> d h c"), writes=[b_w])
        S.dma("pool", wuv[:], W["dsa_w_uv"][j].rearrange("h (cc p) d -> p h cc d", p=128), writes=[b_w])
        S.dma("pool", wout[:], W["dsa_w_out"][j].rearrange("(kc p) n -> p kc n", p=128), writes=[b_w])
        gq = sb("ds_gq", [128, 256], F32)
        gkv = sb("ds_gkv", [128, 256], F32)
        kg = sb("ds_kg", [128, 64], F32)
        kb = sb("ds_kb", [128, 64], F32)
        b_g = Buf("dsg")
        S.dma("sp", gq[:], W["dsa_q_norm_g"][j].partition_broadcast(128), writes=[b_g])
        S.dma("sp", gkv[:], W["dsa_kv_norm_g"][j].partition_broadcast(128), writes=[b_g])
        S.dma("sp", kg[:], W["dsa_kidx_norm_g"][j].partition_broadcast(128), writes=[b_g])
        S.dma("sp", kb[:], W["dsa_kidx_norm_b"][j].partition_broadcast(128), writes=[b_g])
        onesb = sb("ds_ones", [128, 128], BF16)
        b_k = Buf("dsconst")
        S.op("pool", lambda e: e.memset(onesb[:], 1.0), writes=[b_k])
        ckvT = sb("ds_ckvT", [128, 2, SEQ], BF16)
        ckv = sb("ds_ckv", [128, NTS, 256], BF16)
        kidxT = sb("ds_kidxT", [64, SEQ], BF16)
        b_ckvT, b_ckv, b_kidxT = bufs(NTS, "ckvT"), bufs(NTS, "ckv"), bufs(NTS, "kidxT")
        xts = [sb("ds_xt%d" % i, [128, 8, 128], BF16) for i in range(2)]
        b_xts = bufs(2, "dsxt")
        cqT = sb("ds_cqT", [128, 2, 512], BF16)
        b_cqT = bufs(4, "cqT")
        qTh = [sb("ds_qTh%d" % i, [128, 512], BF16) for i in range(2)]
        b_qTh = bufs(2, "qTh")
        qlatT = sb("ds_qlatT", [128, 2, 8, 512], BF16)
        b_qlat = bufs(8, "qlat")
        qidxT = sb("ds_qidxT", [64, 8, 512], BF16)
        b_qidx = bufs(8, "qidx")
        widx = sb("ds_widx", [128, 4, 8], F32)
        b_widx = bufs(4, "widx")
        ss = sb("ds_ss", [128, 8], F32)
        b_ss = Buf("ss")
        junk = sb("ds_junk", [128, 256], BF16)
        b_junk = Buf("junk")
        cq = sb("ds_cq", [128, 256], BF16)
        b_cq = Buf("cq")
        kst = sb("ds_kst", [128, 16], F32)
        b_kst = Buf("kst")
        kx = sb("ds_kx", [128, 64], F32)
        kxb = sb("ds_kxb", [128, 64], BF16)
        b_kx = Buf("kx")
        acc = sb("ds_acc", [128, SEQ], F32)
        work = sb("ds_work", [128, SEQ], F32)
        b_acc, b_work = Buf("acc"), Buf("work")
        rr = [sb("ds_r%d" % i, [128, 512], F32) for i in range(2)]
        b_rr = bufs(2, "r")
        m8 = sb("ds_m8", [128, 8], F32)
        b_m8 = Buf("m8")
        mask = sb("ds_mask", [128, SEQ], BF16)
        b_mask = Buf("mask")
        maskT = sb("ds_maskT", [128, NTS, 128], BF16)
        b_maskT = bufs(NTS // 4, "maskT")
        E = [sb("ds_E%d" % i, [128, 4, 128], BF16) for i in range(2)]
        P = [sb("ds_P%d" % i, [128, 4, 128], BF16) for i in range(2)]
        b_E, b_P = bufs(2, "E"), bufs(2, "P")
        rden = sb("ds_rden", [128, 512], F32)
        b_rden = Buf("rden")
        olat = sb("ds_olat", [128, 2, 4, 128], BF16)
        b_olat = Buf("olat")
        oT = sb("ds_oT", [128, 8, 128], BF16)
        b_oT = bufs(2, "oT")
        pidx = [ps("ds_pidx%d" % i, [128, 512], F32) for i in range(2)]
        b_pidx = bufs(2, "pidx")
        pmt = ps("ds_pmt", [128, 8, 128], BF16)
        b_pmt = Buf("pmt")
        plg = [ps("ds_plg%d" % i, [128, 4, 128], F32) for i in range(2)]
        b_plg = bufs(2, "plg")
        polat = [ps("ds_pol%d" % i, [128, 4, 128], F32) for i in range(2)]
        b_pol = bufs(2, "pol")
        pden = ps("ds_pden", [128, 512], F32)
        b_pden = Buf("pden")
        if os.environ.get("KDBG_NOROUTER"):
            router = None
        epi = Epi(S, nc, st, C, ln_g_row, ln_b_row, router=router, pt=polat, b_pt=b_pol,
                  pr=pden[:, 0:NEXP], b_pr=b_pden, nbuf=1)

        for gb in range(T // 512):
            seq = gb // (SEQ // 512)
            blk = gb % (SEQ // 512)
            for ts in range(4):
                qi = blk * 4 + ts
                tcols = slice(ts * 128, (ts + 1) * 128)
                scols = slice(qi * 128, (qi + 1) * 128)
                xt, b_xt = xts[ts % 2], b_xts[ts % 2]
                S.dma("sp", xt[:], XT_src[gb * 4 + ts], reads=[bXT_src[gb * 4 + ts]], writes=[b_xt])
                pa0, bpa0 = plg[0], b_plg[0]
                pa1, bpa1 = plg[1], b_plg[1]
                pa0f = pa0[:].rearrange("p a b -> p (a b)")
                pa1f = pa1[:].rearrange("p a b -> p (a b)")
                for kc in range(8):
                    S.op("pe", lambda e: e.matmul(pa0f, lhsT=xt[:, kc, :], rhs=win[:, kc, 0:512],
                                                  start=(kc == 0), stop=(kc == 7)), reads=[b_xt, b_w], writes=[bpa0])
                for kc in range(8):
                    S.op("pe", lambda e: e.matmul(pa1f[:, 0:72], lhsT=xt[:, kc, :], rhs=win[:, kc, 512:584],
                                                  start=(kc == 0), stop=(kc == 7)), reads=[b_xt, b_w], writes=[bpa1])
                S.op("act", lambda e: e.activation(out=junk[:], in_=pa0f[:, 0:256], func=AF.Square,
                                                   accum_out=ss[:, 0:1]), reads=[bpa0], writes=[b_junk, b_ss])
                S.op("act", lambda e: e.activation(out=junk[:], in_=pa0f[:, 256:512], func=AF.Square,
                                                   accum_out=ss[:, 1:2]), reads=[bpa0], writes=[b_junk, b_ss])
                S.op("dve", lambda e: e.tensor_scalar(out=ss[:, 2:4], in0=ss[:, 0:2], scalar1=1.0 / 256.0,
                                                      scalar2=float(RMS_EPS), op0=ALU.mult, op1=ALU.add),
                     reads=[b_ss], writes=[b_ss])
                S.op("act", lambda e: e.activation(out=ss[:, 4:6], in_=ss[:, 2:4], func=AF.Ln), reads=[b_ss], writes=[b_ss])
                S.op("act", lambda e: e.activation(out=ss[:, 6:8], in_=ss[:, 4:6], func=AF.Exp, scale=-0.5),
                     reads=[b_ss], writes=[b_ss])
                S.op("dve", lambda e: e.scalar_tensor_tensor(out=cq[:], in0=pa0f[:, 0:256], scalar=ss[:, 6:7],
                                                             in1=gq[:], op0=ALU.mult, op1=ALU.mult),
                     reads=[bpa0, b_ss, b_g], writes=[b_cq])
                S.op("dve", lambda e: e.scalar_tensor_tensor(out=ckv[:, qi, :], in0=pa0f[:, 256:512], scalar=ss[:, 7:8],
                                                             in1=gkv[:], op0=ALU.mult, op1=ALU.mult),
                     reads=[bpa0, b_ss, b_g], writes=[b_ckv[qi]])
                S.op("dve", lambda e: e.bn_stats(out=kst[:, 0:6], in_=pa1f[:, 0:64]), reads=[bpa1], writes=[b_kst])
                S.op("dve", lambda e: e.bn_aggr(out=kst[:, 6:8], in_=kst[:, 0:6]), reads=[b_kst], writes=[b_kst])
                S.op("dve", lambda e: e.tensor_scalar_add(out=kst[:, 8:9], in0=kst[:, 7:8], scalar1=float(LN_EPS)),
                     reads=[b_kst], writes=[b_kst])
                S.op("act", lambda e: e.activation(out=kst[:, 9:10], in_=kst[:, 8:9], func=AF.Ln),
                     reads=[b_kst], writes=[b_kst])
                S.op("act", lambda e: e.activation(out=kst[:, 10:11], in_=kst[:, 9:10], func=AF.Exp, scale=-0.5),
                     reads=[b_kst], writes=[b_kst])
                S.op("dve", lambda e: e.tensor_scalar(out=kst[:, 11:12], in0=kst[:, 6:7], scalar1=-1.0,
                                                      scalar2=kst[:, 10:11], op0=ALU.mult, op1=ALU.mult),
                     reads=[b_kst], writes=[b_kst])
                S.op("act", lambda e: e.activation(out=kx[:], in_=pa1f[:, 0:64], func=AF.Identity,
                                                   scale=kst[:, 10:11], bias=kst[:, 11:12]),
                     reads=[bpa1, b_kst], writes=[b_kx])
                S.op("dve", lambda e: e.tensor_tensor(out=kx[:], in0=kx[:], in1=kg[:], op=ALU.mult),
                     reads=[b_kx, b_g], writes=[b_kx])
                S.op("dve", lambda e: e.tensor_tensor(out=kxb[:], in0=kx[:], in1=kb[:], op=ALU.add),
                     reads=[b_kx, b_g], writes=[b_kx])
                S.op("dve", lambda e: e.tensor_scalar_mul(out=widx[:, ts, :], in0=pa1f[:, 64:72], scalar1=float(IDX_SCALE)),
                     reads=[bpa1], writes=[b_widx[ts]])
                for cc in range(2):
                    S.op("pe", lambda e: e.transpose(pmt[:, cc, :], cq[:, cc * 128:(cc + 1) * 128], C.identb[:]),
                         reads=[b_cq, C.b_const], writes=[b_pmt])
                for cc in range(2):
                    S.op("pe", lambda e: e.transpose(pmt[:, 2 + cc, :], ckv[:, qi, cc * 128:(cc + 1) * 128], C.identb[:]),
                         reads=[b_ckv[qi], C.b_const], writes=[b_pmt])
                S.op("pe", lambda e: e.transpose(pmt[0:64, 4, :], kxb[:], C.identb[:]),
                     reads=[b_kx, C.b_const], writes=[b_pmt])
                S.op("act", lambda e: e.activation(out=cqT[:, :, tcols], in_=pmt[:, 0:2, :], func=AF.Copy),
                     reads=[b_pmt], writes=[b_cqT[ts]])
                S.op("act", lambda e: e.activation(out=ckvT[:, :, scols], in_=pmt[:, 2:4, :], func=AF.Copy),
                     reads=[b_pmt], writes=[b_ckvT[qi]])
                S.op("dve", lambda e: e.tensor_copy(out=kidxT[:, scols], in_=pmt[0:64, 4, :]),
                     reads=[b_pmt], writes=[b_kidxT[qi]])
            for h in range(8):
                pqh, bpqh = pidx[0], b_pidx[0]
                pqi, bpqi = pidx[1], b_pidx[1]
                qb, bqb = qTh[h % 2], b_qTh[h % 2]
                for cc in range(2):
                    S.op("pe", lambda e: e.matmul(pqh[:], lhsT=wuq[:, cc, h * 128:(h + 1) * 128], rhs=cqT[:, cc, :],
                                                  start=(cc == 0), stop=(cc == 1)), reads=[b_w] + b_cqT, writes=[bpqh])
                S.op("act", lambda e: e.activation(out=qb[:], in_=pqh[:], func=AF.Copy), reads=[bpqh], writes=[bqb])
                for cc in range(2):
                    pq_, bpq_ = polat[cc], b_pol[cc]
                    pq_f = pq_[:].rearrange("p a b -> p (a b)")
                    S.op("pe", lambda e: e.matmul(pq_f, lhsT=wuk[:, h, cc * 128:(cc + 1) * 128], rhs=qb[:],
                                                  start=True, stop=True), reads=[b_w, bqb], writes=[bpq_])
                    if cc == 0:
                        S.op("dve", lambda e: e.tensor_copy(out=qlatT[:, cc, h, :], in_=pq_f), reads=[bpq_],
                             writes=[b_qlat[h]])
                    else:
                        S.op("act", lambda e: e.activation(out=qlatT[:, cc, h, :], in_=pq_f, func=AF.Copy),
                             reads=[bpq_], writes=[b_qlat[h]])
                for cc in range(2):
                    S.op("pe", lambda e: e.matmul(pqi[0:64, :], lhsT=wqi[:, cc, h * 64:(h + 1) * 64], rhs=cqT[:, cc, :],
                                                  start=(cc == 0), stop=(cc == 1)), reads=[b_w] + b_cqT, writes=[bpqi])
                S.op("dve", lambda e: e.tensor_copy(out=qidxT[:, h, :], in_=pqi[0:64, :]), reads=[bpqi],
                     writes=[b_qidx[h]])
            for ts in range(4):
                qi = blk * 4 + ts
                tile = gb * 4 + ts
                tcols = slice(ts * 128, (ts + 1) * 128)
                L = (qi + 1) * 128
                epi.prefetch(X_res, bX_res, tile)
                nseg = (L + 511) // 512
                for sg_ in range(nseg):
                    wd_ = min(512, L - sg_ * 512)
                    kcols = slice(sg_ * 512, sg_ * 512 + wd_)
                    kread = b_kidxT[sg_ * 4:sg_ * 4 + (wd_ // 128)]
                    for h in range(8):
                        k2 = (sg_ * 8 + h) % 2
                        S.op("pe", lambda e: e.matmul(pidx[k2][:, 0:wd_], lhsT=qidxT[:, h, tcols], rhs=kidxT[:, kcols],
                                                      start=True, stop=True),
                             reads=[b_qidx[h]] + kread, writes=[b_pidx[k2]])
                        S.op("act", lambda e: e.activation(out=rr[k2][:, 0:wd_], in_=pidx[k2][:, 0:wd_], func=AF.Relu),
                             reads=[b_pidx[k2]], writes=[b_rr[k2]])
                        if h == 0:
                            S.op("dve", lambda e: e.tensor_scalar_mul(out=acc[:, kcols], in0=rr[k2][:, 0:wd_],
                                                                      scalar1=widx[:, ts, 0:1]),
                                 reads=[b_rr[k2], b_widx[ts]], writes=[b_acc])
                        else:
                            S.op("dve", lambda e: e.scalar_tensor_tensor(out=acc[:, kcols], in0=rr[k2][:, 0:wd_],
                                                                         scalar=widx[:, ts, h:h + 1], in1=acc[:, kcols],
                                                                         op0=ALU.mult, op1=ALU.add),
                                 reads=[b_rr[k2], b_widx[ts], b_acc], writes=[b_acc])
                S.op("pool", lambda e: e.affine_select(out=acc[:, qi * 128:L], in_=acc[:, qi * 128:L], pattern=[[-1, 128]],
                                                       compare_op=ALU.is_ge, fill=C.negbig_reg, base=0,
                                                       channel_multiplier=1), reads=[b_acc], writes=[b_acc])
                if qi >= 2:
                    nr = TOPK // 8
                    for r in range(nr):
                        src = acc if r == 0 else work
                        bsrc = b_acc if r == 0 else b_work
                        S.op("dve", lambda e: e.max(out=m8[:], in_=src[:, 0:L]), reads=[bsrc], writes=[b_m8])
                        if r < nr - 1:
                            S.op("dve", lambda e: e.match_replace(out=work[:, 0:L], in_to_replace=m8[:],
                                                                  in_values=src[:, 0:L], imm_value=float(NEG_BIG)),
                                 reads=[bsrc, b_m8], writes=[b_work])
                    S.op("dve", lambda e: e.tensor_scalar(out=mask[:, 0:L], in0=acc[:, 0:L], scalar1=m8[:, 7:8],
                                                          scalar2=None, op0=ALU.is_ge), reads=[b_acc, b_m8], writes=[b_mask])
                else:
                    S.op("dve", lambda e: e.tensor_scalar(out=mask[:, 0:L], in0=acc[:, 0:L], scalar1=-1e29,
                                                          scalar2=None, op0=ALU.is_ge), reads=[b_acc], writes=[b_mask])
                for g4 in range((qi + 4) // 4):
                    n4 = min(4, qi + 1 - g4 * 4)
                    for q4 in range(n4):
                        st_ = g4 * 4 + q4
                        S.op("pe", lambda e: e.transpose(pmt[:, q4, :], mask[:, st_ * 128:(st_ + 1) * 128], C.identb[:]),
                             reads=[b_mask, C.b_const], writes=[b_pmt])
                    S.op("act", lambda e: e.activation(out=maskT[:, g4 * 4:g4 * 4 + n4, :], in_=pmt[:, 0:n4, :],
                                                       func=AF.Copy), reads=[b_pmt], writes=[b_maskT[g4]])
                for hg in range(2):
                    for st_ in range(qi + 1):
                        k2 = st_ % 2
                        plf = plg[k2][:].rearrange("p a b -> p (a b)")
                        for cc in range(2):
                            S.op("pe", lambda e: e.matmul(plg[k2][:], lhsT=ckvT[:, cc, st_ * 128:(st_ + 1) * 128],
                                                          rhs=qlatT[:, cc, hg * 4:(hg + 1) * 4, tcols],
                                                          start=(cc == 0), stop=(cc == 1)),
                                 reads=[b_ckvT[st_]] + b_qlat[hg * 4:(hg + 1) * 4], writes=[b_plg[k2]])
                        S.op("act", lambda e: e.activation(out=E[k2][:], in_=plg[k2][:], func=AF.Exp, scale=float(ATT_SCALE)),
                             reads=[b_plg[k2]], writes=[b_E[k2]])
                        S.op("pool", lambda e: e.tensor_tensor(out=P[k2][:], in0=E[k2][:],
                                                               in1=maskT[:, st_, :].unsqueeze(1).broadcast_to([128, 4, 128]),
                                                               op=ALU.mult),
                             reads=[b_E[k2], b_maskT[st_ // 4]], writes=[b_P[k2]])
                        for cc in range(2):
                            S.op("pe", lambda e: e.matmul(polat[cc][:], lhsT=ckv[:, st_, cc * 128:(cc + 1) * 128], rhs=P[k2][:],
                                                          start=(st_ == 0), stop=(st_ == qi)),
                                 reads=[b_ckv[st_], b_P[k2]], writes=[b_pol[cc]])
                        S.op("pe", lambda e: e.matmul(pden[:], lhsT=onesb[:], rhs=P[k2][:].rearrange("p a b -> p (a b)"),
                                                      start=(st_ == 0), stop=(st_ == qi)),
                             reads=[b_k, b_P[k2]], writes=[b_pden])
                    S.op("dve", lambda e: e.reciprocal(out=rden[:], in_=pden[:]), reads=[b_pden], writes=[b_rden])
                    for cc in range(2):
                        S.op("act", lambda e: e.activation(out=olat[:, cc, :, :], in_=polat[cc][:], func=AF.Copy),
                             reads=[b_pol[cc]], writes=[b_olat])
                    po_, bpo_ = plg[0], b_plg[0]
                    for h4 in range(4):
                        h = hg * 4 + h4
                        for cc in range(2):
                            S.op("pe", lambda e: e.matmul(po_[:, h4, :], lhsT=wuv[:, h, cc, :], rhs=olat[:, cc, h4, :],
                                                          start=(cc == 0), stop=(cc == 1)),
                                 reads=[b_w, b_olat], writes=[bpo_])
                    S.op("dve", lambda e: e.tensor_tensor(out=oT[:, hg * 4:(hg + 1) * 4, :], in0=po_[:],
                                                          in1=rden[:].rearrange("p (a b) -> p a b", a=4), op=ALU.mult),
                         reads=[bpo_, b_rden], writes=[b_oT[hg]])
                for nh in range(2):
                    for h in range(8):
                        S.op("pe", lambda e: e.matmul(pidx[nh][:], lhsT=oT[:, h, :], rhs=wout[:, h, nh * 512:(nh + 1) * 512],
                                                      start=(h == 0), stop=(h == 7)),
                             reads=[b_oT[h // 4], b_w], writes=[b_pidx[nh]])
                epi.run(tile, [pidx[0][:], pidx[1][:]], [b_pidx[0], b_pidx[1]], X_dst, bX_dst, XT_dst, bXT_dst,
                        gates_dst=Gt, bG=bG)
        S.barrier()


def emit_consts(S, nc, st, C):
    C.ident = st.enter_context(nc.sbuf_tensor("c_ident", [128, 128], F32))
    C.identb = st.enter_context(nc.sbuf_tensor("c_identb", [128, 128], BF16))
    C.b_const = Buf("const")
    C.negbig_reg = nc.gpsimd.to_reg(float(NEG_BIG))
    S.op("pool", lambda e: e.memset(C.ident[:], 0.0), writes=[C.b_const])
    S.op("pool", lambda e: e.affine_select(out=C.ident[:], in_=C.ident[:], pattern=[[-1, 128]],
                                           compare_op=ALU.not_equal, fill=1.0, base=0, channel_multiplier=1),
         reads=[C.b_const], writes=[C.b_const])
    S.op("pool", lambda e: e.tensor_copy(out=C.identb[:], in_=C.ident[:]), reads=[C.b_const], writes=[C.b_const])


WEIGHT_SPECS = [
    ("ln_g", [4, 2, 1024]), ("ln_b", [4, 2, 1024]),
    ("hg_w_in", [2, 1024, 4096]), ("hg_lower_bounds", [2, 1024]), ("hg_norm_g", [2, 8, 128]),
    ("hg_w_out", [2, 1024, 1024]),
    ("dsa_w_in", [2, 1024, 584]), ("dsa_q_norm_g", [2, 256]), ("dsa_kv_norm_g", [2, 256]),
    ("dsa_w_uq", [2, 256, 1024]), ("dsa_w_uk", [2, 8, 128, 256]), ("dsa_w_uv", [2, 8, 256, 128]),
    ("dsa_w_qidx", [2, 256, 512]), ("dsa_kidx_norm_g", [2, 64]), ("dsa_kidx_norm_b", [2, 64]),
    ("dsa_w_out", [2, 1024, 1024]),
    ("ffn_w_gate_up", [2, NFC, 128, 2 * 8 * FC]), ("ffn_w_down", [2, NFC, 128, (FC // 128) * 1024]),
    ("moe_w_router", [2, 128, 8 * 8]), ("moe_w_gate_up", [2, 8, NFC, 128, 2 * 8 * FC]),
    ("moe_w_down", [2, 8, NFC, 128, (FC // 128) * 1024]),
]


def build_program(plan=None):
    if plan is None:
        plan = list(range(8))
    nc = bass.Bass("TRN2", target_bir_lowering=False)
    x_in = nc.dram_tensor("x", [T, D], F32, kind="ExternalInput").ap()
    W = {name: nc.dram_tensor(name, shape, F32, kind="ExternalInput").ap() for name, shape in WEIGHT_SPECS}
    out = nc.dram_tensor("out", [T, D], F32, kind="ExternalOutput").ap()
    Xs = nc.dram_tensor("Xs", [T, D], F32, kind="Internal").ap()
    XT = [nc.dram_tensor("XT%d" % i, [NT, 128, 8, 128], BF16, kind="Internal").ap() for i in range(2)]
    Gt = nc.dram_tensor("Gt", [T, NEXP], F32, kind="Internal").ap()
    bX_in = bufs(NT, "xin")
    bXs = bufs(NT, "Xs")
    bXT = [bufs(NT, "XT0_"), bufs(NT, "XT1_")]
    bG = bufs(NT, "G")
    bOut = bufs(NT, "out")
    C = Ctx()
    with ExitStack() as st:
        S = Sched(nc, st)
        emit_consts(S, nc, st, C)
        cur_X, cur_bX = x_in, bX_in
        cur = 0
        phase_prep(S, nc, C, x_in, bX_in, XT[0], bXT[0])
        for si, sub in enumerate(plan):
            layer, kind = sub // 2, sub % 2
            j = layer // 2
            last = si == len(plan) - 1
            X_dst, bX_dst = (out, bOut) if last else (Xs, bXs)
            XT_dst, bXT_dst = (None, None) if last else (XT[1 - cur], bXT[1 - cur])
            if kind == 1:
                if layer % 2 == 0:
                    experts = [(W["ffn_w_gate_up"][j], W["ffn_w_down"][j])]
                    gates = None
                else:
                    experts = [(W["moe_w_gate_up"][j, e], W["moe_w_down"][j, e])
                               for e in range(int(os.environ.get("KDBG_NEXP", NEXP)))]
                    gates = Gt
                phase_ffn(S, nc, C, XT[cur], bXT[cur], cur_X, cur_bX, X_dst, bX_dst, XT_dst, bXT_dst,
                          experts, gates, bG, W["ln_g"][layer, 1], W["ln_b"][layer, 1])
            elif layer % 2 == 0:
                phase_hgrn(S, nc, C, j, W, XT[cur], bXT[cur], cur_X, cur_bX, X_dst, bX_dst, XT_dst, bXT_dst,
                           W["ln_g"][layer, 0], W["ln_b"][layer, 0])
            else:
                phase_dsa(S, nc, C, j, W, XT[cur], bXT[cur], cur_X, cur_bX, X_dst, bX_dst, XT_dst, bXT_dst,
                          W["ln_g"][layer, 0], W["ln_b"][layer, 0], W["moe_w_router"][j], Gt, bG)
            cur_X, cur_bX = X_dst, bX_dst
            cur = 1 - cur
        S.barrier()
        C.ninst = S.ninst
    return nc


def relayout_weights(inputs):
    out = {}
    for name, _ in WEIGHT_SPECS:
        w = np.asarray(inputs[name], dtype=np.float32)
        if name in ("ffn_w_gate_up", "moe_w_gate_up"):
            lead = w.shape[:-2]
            w = w.reshape(lead + (8, 128, 2, NFC, FC))
            nl = len(lead)
            perm = tuple(range(nl)) + (nl + 3, nl + 1, nl + 2, nl + 0, nl + 4)
            w = w.transpose(perm).reshape(lead + (NFC, 128, 2 * 8 * FC))
        elif name == "moe_w_router":
            w = w.reshape(2, 8, 128, NEXP).transpose(0, 2, 1, 3).reshape(2, 128, 8 * NEXP)
        elif name in ("ffn_w_down", "moe_w_down"):
            lead = w.shape[:-2]
            w = w.reshape(lead + (NFC, FC // 128, 128, D))
            nl = len(lead)
            perm = tuple(range(nl)) + (nl + 0, nl + 2, nl + 1, nl + 3)
            w = w.transpose(perm).reshape(lead + (NFC, 128, (FC // 128) * D))
        out[name] = np.ascontiguousarray(w)
    return out


_PROG = {}


def kernel(**inputs):
    x = np.ascontiguousarray(inputs["x"], dtype=np.float32)
    if "full" not in _PROG:
        _PROG["full"] = build_program()
    nc = _PROG["full"]
    wmap = relayout_weights(inputs)
    in_maps = []
    for c in range(8):
        m = dict(wmap)
        m["x"] = x[c * NSEQ:(c + 1) * NSEQ].reshape(T, D)
        in_maps.append(m)
    res = run_bass_kernel_spmd(nc, in_maps, core_ids=list(range(8)))
    outs = [np.asarray(r["out"]).reshape(NSEQ, SEQ, D) for r in res.results]
    return np.concatenate(outs, axis=0).astype(np.float32)
```

```python
from contextlib import ExitStack
import numpy as np
import concourse.bass as bass
import concourse.mybir as mybir
from concourse.bass_utils import run_bass_kernel_spmd

F32 = mybir.dt.float32
BF16 = mybir.dt.bfloat16
AF = mybir.ActivationFunctionType
ALU = mybir.AluOpType
AX = mybir.AxisListType

D = 1024
SEQ = 2048
NSEQ = 2
T = NSEQ * SEQ
NT = T // 128
NTS = SEQ // 128
DEPTH = 4
DFF = 3584
NEXP = 8
ALPHA = (2 * DEPTH) ** 0.25
LN_EPS = 1e-5
RMS_EPS = 1e-6
NEG_BIG = -1e30
FC = 512
NFC = DFF // FC

NDMA = 40
NDMA_SP = 28


class Buf:
    __slots__ = ("name", "w", "r")

    def __init__(self, name=""):
        self.name = name
        self.w = None
        self.r = {}


class Sched:
    def __init__(self, nc, stack):
        self.nc = nc
        self.engs = {"pe": nc.tensor, "act": nc.scalar, "dve": nc.vector,
                     "pool": nc.gpsimd, "sp": nc.sync}
        self.sem = {k: stack.enter_context(nc.semaphore("s_" + k)) for k in self.engs}
        self.cnt = {k: 0 for k in self.engs}
        self.seen = {k: {o: 0 for o in self.engs} for k in self.engs}
        self.dsem = [stack.enter_context(nc.semaphore("d%d" % i)) for i in range(NDMA)]
        self.dcnt = [0] * NDMA
        self.dseen = {k: [0] * NDMA for k in self.engs}
        self.rr = 0
        self.rrs = {}
        self.ninst = 0

    def _wait(self, en, tok):
        eng = self.engs[en]
        if tok[0] == "e":
            _, e2, idx = tok
            if e2 == en and en == "pe":
                return
            if self.seen[en][e2] >= idx:
                return
            eng.wait_ge(self.sem[e2], idx)
            self.seen[en][e2] = idx
        else:
            _, j, val = tok
            if self.dseen[en][j] >= val:
                return
            eng.wait_ge(self.dsem[j], val)
            self.dseen[en][j] = val

    def _deps(self, en, reads, writes):
        for b in reads:
            if b.w is not None:
                self._wait(en, b.w)
        for b in writes:
            if b.w is not None:
                self._wait(en, b.w)
            for t in b.r.values():
                self._wait(en, t)

    def _commit(self, tok, reads, writes):
        key = tok[1] if tok[0] == "e" else ("d", tok[1])
        for b in reads:
            b.r[key] = tok
        for b in writes:
            b.w = tok
            b.r = {}

    def op(self, en, fn, reads=(), writes=()):
        self._deps(en, reads, writes)
        ins = fn(self.engs[en])
        self.cnt[en] += 1
        ins.then_inc(self.sem[en], 1)
        self.ninst += 1
        self._commit(("e", en, self.cnt[en]), reads, writes)

    def dma(self, en, out, in_, reads=(), writes=(), **kw):
        lo, hi = (0, NDMA_SP) if en == "sp" else (NDMA_SP, NDMA)
        j = self.rrs.get(en, lo)
        self.rrs[en] = lo + (j + 1 - lo) % (hi - lo)
        self._deps(en, reads, writes)
        if self.dcnt[j] > 0:
            self._wait(en, ("d", j, self.dcnt[j]))
        ins = self.engs[en].dma_start(out=out, in_=in_, **kw)
        self.dcnt[j] += 16
        ins.then_inc(self.dsem[j], 16)
        self.ninst += 1
        self._commit(("d", j, self.dcnt[j]), reads, writes)

    def barrier(self):
        for en in self.engs:
            for e2 in self.engs:
                if self.cnt[e2] > 0:
                    self._wait(en, ("e", e2, self.cnt[e2]))
            for j in range(NDMA):
                if self.dcnt[j] > 0:
                    self._wait(en, ("d", j, self.dcnt[j]))


class Ctx:
    pass


_UID = [0]


def uq(name):
    _UID[0] += 1
    return "%s_u%d" % (name, _UID[0])


def bufs(n, name=""):
    return [Buf("%s%d" % (name, i)) for i in range(n)]


class Epi:
    def __init__(self, S, nc, st, C, ln_g_row, ln_b_row, router=None, pt=None, b_pt=None, pr=None, b_pr=None, nbuf=2):
        self.S, self.nc, self.C = S, nc, C
        sb = lambda name, shape, dt: st.enter_context(nc.sbuf_tensor(uq(name), shape, dt))
        self.nbuf = nbuf
        self.xres = [sb("ep_xres%d" % i, [128, D], F32) for i in range(nbuf)]
        self.b_xres = bufs(nbuf, "xres")
        self.gbc = sb("ep_g", [128, D], F32)
        self.bbc = sb("ep_b", [128, D], F32)
        self.b_gb = Buf("gb")
        self.stats = sb("ep_stats", [128, 12], F32)
        self.mv = sb("ep_mv", [128, 8], F32)
        self.b_small = Buf("small")
        self.xT = [sb("ep_xT%d" % i, [128, 8, 128], BF16) for i in range(nbuf)]
        self.b_xT = bufs(nbuf, "xT")
        if pt is None:
            self.pt = [st.enter_context(nc.psum_tensor(uq("ep_pt%d" % i), [128, 4, 128], F32)) for i in range(2)]
            self.b_pt = bufs(2, "ept")
        else:
            self.pt, self.b_pt = pt, b_pt
        self.k = 0
        self.pk = 0
        S.dma("sp", self.gbc[:], ln_g_row.partition_broadcast(128), writes=[self.b_gb])
        S.dma("sp", self.bbc[:], ln_b_row.partition_broadcast(128), writes=[self.b_gb])
        self.router = router
        if router is not None:
            self.wr = sb("ep_wr", [128, 8, NEXP], F32)
            self.b_wr = Buf("wr")
            S.dma("sp", self.wr[:], router.rearrange("p (kc e) -> p kc e", e=NEXP), writes=[self.b_wr])
            self.xTf = sb("ep_xTf", [128, 8, 128], F32)
            self.b_xTf = Buf("xTf")
            if pr is None:
                self.pr = st.enter_context(nc.psum_tensor(uq("ep_pr"), [128, NEXP], F32))[:]
                self.b_pr = Buf("pr")
            else:
                self.pr, self.b_pr = pr, b_pr
            self.rt = sb("ep_rt", [128, 6, NEXP], F32)
            self.b_rt = Buf("rt")

    def prefetch(self, X_src, bX_src, tile):
        i = self.pk % self.nbuf
        self.pk += 1
        self.S.dma("sp", self.xres[i][:], X_src[tile * 128:(tile + 1) * 128, :],
                   reads=[bX_src[tile]], writes=[self.b_xres[i]])

    def run(self, tile, y_ap, y_bufs, X_dst, bX_dst, XT_dst, bXT_dst, gates_dst=None, bG=None):
        S, C = self.S, self.C
        i = self.k % self.nbuf
        self.k += 1
        xr, bx = self.xres[i], self.b_xres[i]
        st_, mv, bs = self.stats, self.mv, self.b_small
        if isinstance(y_ap, (list, tuple)):
            for hh, (yh, yb) in enumerate(zip(y_ap, y_bufs)):
                S.op("dve", lambda e: e.scalar_tensor_tensor(out=xr[:, hh * 512:(hh + 1) * 512],
                                                             in0=xr[:, hh * 512:(hh + 1) * 512], scalar=float(ALPHA),
                                                             in1=yh, op0=ALU.mult, op1=ALU.add),
                     reads=[yb, bx], writes=[bx])
        else:
            S.op("dve", lambda e: e.scalar_tensor_tensor(out=xr[:], in0=xr[:], scalar=float(ALPHA), in1=y_ap,
                                                         op0=ALU.mult, op1=ALU.add),
                 reads=list(y_bufs) + [bx], writes=[bx])
        S.op("dve", lambda e: e.bn_stats(out=st_[:, 0:6], in_=xr[:, 0:512]), reads=[bx], writes=[bs])
        S.op("dve", lambda e: e.bn_stats(out=st_[:, 6:12], in_=xr[:, 512:1024]), reads=[bx], writes=[bs])
        S.op("dve", lambda e: e.bn_aggr(out=mv[:, 0:2], in_=st_[:, 0:12]), reads=[bs], writes=[bs])
        S.op("dve", lambda e: e.tensor_scalar_add(out=mv[:, 2:3], in0=mv[:, 1:2], scalar1=float(LN_EPS)),
             reads=[bs], writes=[bs])
        S.op("act", lambda e: e.activation(out=mv[:, 3:4], in_=mv[:, 2:3], func=AF.Ln), reads=[bs], writes=[bs])
        S.op("act", lambda e: e.activation(out=mv[:, 4:5], in_=mv[:, 3:4], func=AF.Exp, scale=-0.5),
             reads=[bs], writes=[bs])
        S.op("dve", lambda e: e.tensor_scalar(out=mv[:, 5:6], in0=mv[:, 0:1], scalar1=-1.0, scalar2=mv[:, 4:5],
                                              op0=ALU.mult, op1=ALU.mult), reads=[bs], writes=[bs])
        S.op("act", lambda e: e.activation(out=xr[:], in_=xr[:], func=AF.Identity, scale=mv[:, 4:5],
                                           bias=mv[:, 5:6]), reads=[bs, bx], writes=[bx])
        S.op("pool", lambda e: e.tensor_tensor(out=xr[:], in0=xr[:], in1=self.gbc[:], op=ALU.mult),
             reads=[bx, self.b_gb], writes=[bx])
        S.op("pool", lambda e: e.tensor_tensor(out=xr[:], in0=xr[:], in1=self.bbc[:], op=ALU.add),
             reads=[bx, self.b_gb], writes=[bx])
        if X_dst is not None:
            S.dma("sp", X_dst[tile * 128:(tile + 1) * 128, :], xr[:], reads=[bx], writes=[bX_dst[tile]])
        if XT_dst is None:
            return
        xT, bxT = self.xT[i], self.b_xT[i]
        for half in range(2):
            pt, bpt = self.pt[half], self.b_pt[half]
            for q in range(4):
                kc = half * 4 + q
                S.op("pe", lambda e: e.transpose(pt[:, q, :], xr[:, kc * 128:(kc + 1) * 128], C.ident[:]),
                     reads=[bx, C.b_const], writes=[bpt])
            if self.router is not None:
                S.op("act", lambda e: e.activation(out=self.xTf[:, half * 4:(half + 1) * 4, :], in_=pt[:], func=AF.Copy),
                     reads=[bpt], writes=[self.b_xTf])
            S.op("act", lambda e: e.activation(out=xT[:, half * 4:(half + 1) * 4, :], in_=pt[:], func=AF.Copy),
                 reads=[bpt], writes=[bxT])
        S.dma("sp", XT_dst[tile], xT[:], reads=[bxT], writes=[bXT_dst[tile]])
        if self.router is not None:
            rt, brt = self.rt, self.b_rt
            for kc in range(8):
                S.op("pe", lambda e: e.matmul(self.pr, lhsT=self.xTf[:, kc, :], rhs=self.wr[:, kc, :],
                                              start=(kc == 0), stop=(kc == 7)),
                     reads=[self.b_xTf, self.b_wr], writes=[self.b_pr])
            S.op("dve", lambda e: e.tensor_copy(out=rt[:, 0, :], in_=self.pr), reads=[self.b_pr], writes=[brt])
            S.op("dve", lambda e: e.max(out=rt[:, 1, :], in_=rt[:, 0, :]), reads=[brt], writes=[brt])
            S.op("dve", lambda e: e.tensor_scalar_mul(out=rt[:, 2, 0:1], in0=rt[:, 1, 0:1], scalar1=-1.0),
                 reads=[brt], writes=[brt])
            S.op("act", lambda e: e.activation(out=rt[:, 3, :], in_=rt[:, 0, :], func=AF.Exp, bias=rt[:, 2, 0:1],
                                               scale=1.0), reads=[brt], writes=[brt])
            S.op("dve", lambda e: e.scalar_tensor_tensor(out=rt[:, 4, :], in0=rt[:, 0, :], scalar=rt[:, 1, 1:2],
                                                         in1=rt[:, 3, :], op0=ALU.is_ge, op1=ALU.mult),
                 reads=[brt], writes=[brt])
            S.op("dve", lambda e: e.reduce_sum(out=rt[:, 2, 1:2], in_=rt[:, 4, :], axis=AX.X),
                 reads=[brt], writes=[brt])
            S.op("dve", lambda e: e.reciprocal(out=rt[:, 2, 2:3], in_=rt[:, 2, 1:2]), reads=[brt], writes=[brt])
            S.op("dve", lambda e: e.tensor_scalar_mul(out=rt[:, 5, :], in0=rt[:, 4, :], scalar1=rt[:, 2, 2:3]),
                 reads=[brt], writes=[brt])
            S.dma("sp", gates_dst[tile * 128:(tile + 1) * 128, :], rt[:, 5, :], reads=[brt], writes=[bG[tile]])


def phase_prep(S, nc, C, X_src, bX_src, XT_dst, bXT_dst):
    with ExitStack() as st:
        sb = lambda name, shape, dt: st.enter_context(nc.sbuf_tensor(uq(name), shape, dt))
        xin = [sb("pp_x%d" % i, [128, D], F32) for i in range(2)]
        b_xin = bufs(2)
        xT = [sb("pp_xT%d" % i, [128, 8, 128], BF16) for i in range(2)]
        b_xT = bufs(2)
        pt = [st.enter_context(nc.psum_tensor(uq("pp_pt%d" % i), [128, 4, 128], F32)) for i in range(2)]
        b_pt = bufs(2)
        for tile in range(NT):
            i = tile % 2
            S.dma("sp", xin[i][:], X_src[tile * 128:(tile + 1) * 128, :], reads=[bX_src[tile]], writes=[b_xin[i]])
            for half in range(2):
                for q in range(4):
                    kc = half * 4 + q
                    S.op("pe", lambda e: e.transpose(pt[half][:, q, :], xin[i][:, kc * 128:(kc + 1) * 128],
                                                     C.ident[:]),
                         reads=[b_xin[i], C.b_const], writes=[b_pt[half]])
                eng = "act" if half == 0 else "dve"
                if eng == "act":
                    S.op("act", lambda e: e.activation(out=xT[i][:, half * 4:(half + 1) * 4, :], in_=pt[half][:],
                                                       func=AF.Copy), reads=[b_pt[half]], writes=[b_xT[i]])
                else:
                    S.op("dve", lambda e: e.tensor_copy(out=xT[i][:, half * 4:(half + 1) * 4, :], in_=pt[half][:]),
                         reads=[b_pt[half]], writes=[b_xT[i]])
            S.dma("sp", XT_dst[tile], xT[i][:], reads=[b_xT[i]], writes=[bXT_dst[tile]])
        S.barrier()


def phase_ffn(S, nc, C, XT_src, bXT_src, X_res, bX_res, X_dst, bX_dst, XT_dst, bXT_dst,
              experts, gates, bG, ln_g_row, ln_b_row):
    moe = gates is not None
    with ExitStack() as st:
        sb = lambda name, shape, dt: st.enter_context(nc.sbuf_tensor(uq(name), shape, dt))
        yacc = sb("ff_y", [128, NTS, D], F32)
        b_y = bufs(NTS, "y")
        xt = sb("ff_xt", [128, NTS, 8, 128], BF16)
        b_xt = bufs(4, "xt")
        NW = 2
        wgu = [sb("ff_wgu%d" % i, [128, 2, 8, FC], BF16) for i in range(NW)]
        wg = [w_[:, 0, :, :] for w_ in wgu]
        wu = [w_[:, 1, :, :] for w_ in wgu]
        wd = [sb("ff_wd%d" % i, [128, FC // 128, D], BF16) for i in range(NW)]
        b_w = bufs(NW, "w")
        NFS = FC // 128
        hT = [sb("ff_h%d" % i, [128, NFS, 512], BF16) for i in range(2)]
        b_h = bufs(2, "h")
        sg = [sb("ff_s%d" % i, [128, 512], BF16) for i in range(2)]
        b_s = bufs(2, "s")
        if moe:
            gt = sb("ff_gt", [128, NTS, NEXP], F32)
            b_gt = Buf("gt")
        pg = [st.enter_context(nc.psum_tensor(uq("ff_pg%d" % i), [128, 512], F32)) for i in range(2)]
        pu = [st.enter_context(nc.psum_tensor(uq("ff_pu%d" % i), [128, 512], F32)) for i in range(2)]
        b_pg, b_pu = bufs(2, "pg"), bufs(2, "pu")
        py = [st.enter_context(nc.psum_tensor(uq("ff_py%d" % i), [128, 512], F32)) for i in range(2)]
        b_py = bufs(2, "py")
        epi = Epi(S, nc, st, C, ln_g_row, ln_b_row)

        for seq in range(NSEQ):
            t0 = seq * SEQ
            for blk in range(4):
                S.dma("sp", xt[:, blk * 4:(blk + 1) * 4, :, :],
                      XT_src[t0 // 128 + blk * 4:t0 // 128 + (blk + 1) * 4].rearrange("n p kc t -> p n kc t"),
                      reads=[bXT_src[(t0 + blk * 512) // 128 + q] for q in range(4)], writes=[b_xt[blk]])
            if moe:
                for q in range(NTS):
                    S.dma("sp", gt[:, q, :], gates[t0 + q * 128:t0 + (q + 1) * 128, :],
                          reads=[bG[t0 // 128 + q]], writes=[b_gt])
            chunks = [(e, fc) for e in range(len(experts)) for fc in range(NFC)]
            items = [(ci, tb) for ci in range(len(chunks)) for tb in range(4)]

            def load_w(ci):
                e, fc = chunks[ci]
                w_gu, w_d = experts[e]
                sl = ci % NW
                S.dma("pool", wgu[sl][:].rearrange("p a k f -> p (a k f)").rearrange("p (c f) -> p c f", f=2048),
                      w_gu[fc].rearrange("p (c f) -> p c f", f=2048), writes=[b_w[sl]])
                S.dma("pool", wd[sl][:].rearrange("p a n -> p (a n)").rearrange("p (c f) -> p c f", f=2048),
                      w_d[fc].rearrange("p (c f) -> p c f", f=2048), writes=[b_w[sl]])

            def up(k):
                ci, tb = items[k]
                sl = ci % NW
                h, bh = hT[k % 2], b_h[k % 2]
                for fs in range(NFS):
                    j = (k * NFS + fs) % 2
                    for kc in range(8):
                        S.op("pe", lambda e: e.matmul(pg[j][:], lhsT=wg[sl][:, kc, fs * 128:(fs + 1) * 128],
                                                      rhs=xt[:, tb * 4:(tb + 1) * 4, kc, :],
                                                      start=(kc == 0), stop=(kc == 7)),
                             reads=[b_w[sl], b_xt[tb]], writes=[b_pg[j]])
                    for kc in range(8):
                        S.op("pe", lambda e: e.matmul(pu[j][:], lhsT=wu[sl][:, kc, fs * 128:(fs + 1) * 128],
                                                      rhs=xt[:, tb * 4:(tb + 1) * 4, kc, :],
                                                      start=(kc == 0), stop=(kc == 7)),
                             reads=[b_w[sl], b_xt[tb]], writes=[b_pu[j]])
                    S.op("act", lambda e: e.activation(out=sg[j][:], in_=pg[j][:], func=AF.Silu),
                         reads=[b_pg[j]], writes=[b_s[j]])
                    S.op("dve", lambda e: e.tensor_tensor(out=h[:, fs, :], in0=sg[j][:], in1=pu[j][:], op=ALU.mult),
                         reads=[b_s[j], b_pu[j]], writes=[bh])

            def down(k):
                ci, tb = items[k]
                e_idx, fc = chunks[ci]
                sl = ci % NW
                h, bh = hT[k % 2], b_h[k % 2]
                for ts in range(4):
                    tl = tb * 4 + ts
                    for nh in range(2):
                        j = (ts * 2 + nh) % 2
                        for fs in range(NFS):
                            S.op("pe", lambda e: e.matmul(py[j][:], lhsT=h[:, fs, ts * 128:(ts + 1) * 128],
                                                          rhs=wd[sl][:, fs, nh * 512:(nh + 1) * 512],
                                                          start=(fs == 0), stop=(fs == NFS - 1)),
                                 reads=[bh, b_w[sl]], writes=[b_py[j]])
                        ya = yacc[:, tl, nh * 512:(nh + 1) * 512]
                        if moe:
                            gsc = gt[:, tl, e_idx:e_idx + 1]
                            if ci == 0:
                                S.op("dve", lambda e: e.tensor_scalar_mul(out=ya, in0=py[j][:], scalar1=gsc),
                                     reads=[b_py[j], b_gt], writes=[b_y[tl]])
                            else:
                                S.op("dve", lambda e: e.scalar_tensor_tensor(out=ya, in0=py[j][:], scalar=gsc, in1=ya,
                                                                             op0=ALU.mult, op1=ALU.add),
                                     reads=[b_py[j], b_gt, b_y[tl]], writes=[b_y[tl]])
                        else:
                            if ci == 0:
                                S.op("dve", lambda e: e.tensor_copy(out=ya, in_=py[j][:]),
                                     reads=[b_py[j]], writes=[b_y[tl]])
                            else:
                                S.op("dve", lambda e: e.tensor_tensor(out=ya, in0=py[j][:], in1=ya, op=ALU.add),
                                     reads=[b_py[j], b_y[tl]], writes=[b_y[tl]])

            load_w(0)
            for k in range(len(items)):
                ci, tb = items[k]
                if tb == 0 and ci + 1 < len(chunks):
                    load_w(ci + 1)
                if k == 0:
                    up(0)
                if k + 1 < len(items):
                    up(k + 1)
                down(k)
            epi.prefetch(X_res, bX_res, t0 // 128)
            for tl in range(NTS):
                tile = t0 // 128 + tl
                if tl + 1 < NTS:
                    epi.prefetch(X_res, bX_res, tile + 1)
                epi.run(tile, yacc[:, tl, :], [b_y[tl]], X_dst, bX_dst, XT_dst, bXT_dst)
        S.barrier()


def phase_hgrn(S, nc, C, j, W, XT_src, bXT_src, X_res, bX_res, X_dst, bX_dst, XT_dst, bXT_dst,
               ln_g_row, ln_b_row):
    w_in_d, w_out_d = W["hg_w_in"][j], W["hg_w_out"][j]
    with ExitStack() as st:
        sb = lambda name, shape, dt: st.enter_context(nc.sbuf_tensor(uq(name), shape, dt))
        ps = lambda name, shape, dt: st.enter_context(nc.psum_tensor(uq(name), shape, dt))
        w_in = sb("hg_win", [128, 8, 4096], BF16)
        w_out = sb("hg_wout", [128, 8, D], BF16)
        b_w = Buf("hgw")
        for sec in range(4):
            S.dma("pool", w_in[:, :, sec * 1024:(sec + 1) * 1024],
                  w_in_d[:, sec * 1024:(sec + 1) * 1024].rearrange("(kc p) n -> p kc n", p=128), writes=[b_w])
        S.dma("pool", w_out[:], w_out_d.rearrange("(kc p) n -> p kc n", p=128), writes=[b_w])
        sc = sb("hg_sc", [128, 5, 8], F32)
        b_sc = Buf("hgsc")
        S.dma("sp", sc[:, 4, :], W["hg_norm_g"][j].rearrange("h p -> p h"), writes=[b_sc],
              allow_slow_non_contiguous=True)
        if j == 0:
            S.op("dve", lambda e: e.memset(sc[:, 2, :], 0.0), writes=[b_sc])
            S.op("dve", lambda e: e.memset(sc[:, 3, :], 1.0), writes=[b_sc])
        else:
            S.dma("sp", sc[:, 0, :], W["hg_lower_bounds"][0].rearrange("(h p) -> p h", p=128), writes=[b_sc],
                  allow_slow_non_contiguous=True)
            S.dma("sp", sc[:, 1, :], W["hg_lower_bounds"][1].rearrange("(h p) -> p h", p=128), writes=[b_sc],
                  allow_slow_non_contiguous=True)
            S.op("dve", lambda e: e.tensor_tensor(out=sc[:, 1, :], in0=sc[:, 1, :], in1=sc[:, 0, :], op=ALU.subtract),
                 reads=[b_sc], writes=[b_sc])
            S.op("act", lambda e: e.activation(out=sc[:, 2, :], in_=sc[:, 1, :], func=AF.Sigmoid),
                 reads=[b_sc], writes=[b_sc])
            S.op("dve", lambda e: e.tensor_scalar(out=sc[:, 3, :], in0=sc[:, 2, :], scalar1=-1.0, scalar2=1.0,
                                                  op0=ALU.mult, op1=ALU.add), reads=[b_sc], writes=[b_sc])
        rmask = sb("hg_rmask", [128, 8, 64], F32)
        cmask = sb("hg_cmask", [64, 64], F32)
        ones = sb("hg_ones", [128, 128], F32)
        epsb = sb("hg_eps", [128, 1], F32)
        b_k = Buf("hgconst")
        S.op("pool", lambda e: e.memset(rmask[:], 1.0), writes=[b_k])
        S.op("pool", lambda e: e.memset(rmask[:, :, 0:1], 0.0), reads=[b_k], writes=[b_k])
        S.op("pool", lambda e: e.memset(cmask[:], 1.0), reads=[b_k], writes=[b_k])
        S.op("pool", lambda e: e.affine_select(out=cmask[:], in_=cmask[:], pattern=[[1, 64]], compare_op=ALU.is_ge,
                                               fill=0.0, base=0, channel_multiplier=-1), reads=[b_k], writes=[b_k])
        S.op("pool", lambda e: e.memset(ones[:], 1.0), reads=[b_k], writes=[b_k])
        S.op("pool", lambda e: e.memset(epsb[:], float(RMS_EPS)), reads=[b_k], writes=[b_k])
        xt = [sb("hg_xt%d" % i, [128, 4, 8, 128], BF16) for i in range(2)]
        b_xt = bufs(2, "hgxt")
        f_ = sb("hg_f", [128, 512], F32)
        lf = sb("hg_lf", [128, 512], F32)
        bb = sb("hg_b", [128, 512], F32)
        eb = sb("hg_eb", [128, 512], F32)
        enb = sb("hg_enb", [128, 512], F32)
        b_f, b_lf, b_bb, b_eb, b_enb = [Buf(n) for n in ("f", "lf", "bb", "eb", "enb")]
        qn = sb("hg_qn", [128, 512], BF16)
        kn = sb("hg_kn", [128, 512], BF16)
        sgt = sb("hg_sgt", [128, 512], BF16)
        b_qn, b_kn, b_sgt = Buf("qn"), Buf("kn"), Buf("sgt")
        v_sb = sb("hg_v", [64, 8, 128], BF16)
        kn_tok = sb("hg_kntok", [64, 8, 128], BF16)
        b_v, b_kntok = Buf("v"), Buf("kntok")
        scm = [sb("hg_scm%d" % i, [64, 64], BF16) for i in range(2)]
        b_scm = bufs(2, "scm")
        Sf = sb("hg_Sf", [128, 8, 128], F32)
        Sbf = sb("hg_Sbf", [128, 8, 128], BF16)
        b_Sf, b_Sbf = bufs(8, "Sf"), bufs(8, "Sbf")
        sq = sb("hg_sq", [128, 512], F32)
        rstd = sb("hg_rstd", [128, 512], F32)
        on = sb("hg_on", [128, 512], F32)
        b_sq, b_rstd, b_on = Buf("sq"), Buf("rstd"), Buf("on")
        onT = sb("hg_onT", [128, 8, 512], BF16)
        b_onT = bufs(8, "onT")
        ymix = sb("hg_ymix", [128, D], F32)
        b_ymix = Buf("ymix")
        pq, pf, pg = ps("hg_pq", [128, 512], F32), ps("hg_pf", [128, 512], F32), ps("hg_pg", [128, 512], F32)
        b_pq, b_pf, b_pg = Buf("pq"), Buf("pf"), Buf("pg")
        pv = [ps("hg_pv%d" % i, [128, 4, 128], F32) for i in range(2)]
        b_pv = bufs(2, "pv")
        po = ps("hg_po", [128, 512], F32)
        b_po = Buf("po")
        pmisc = ps("hg_pmisc", [128, 512], F32)
        b_psc, b_pkv = bufs(2, "psc"), Buf("pkv")
        ptr = ps("hg_ptr", [64, 8, 128], BF16)
        b_ptr = Buf("ptr")
        epi = Epi(S, nc, st, C, ln_g_row, ln_b_row, pt=pv, b_pt=b_pv)

        def load_xt(gb):
            i = gb % 2
            S.dma("sp", xt[i][:], XT_src[gb * 4:(gb + 1) * 4].rearrange("n p kc t -> p n kc t"),
                  reads=[bXT_src[gb * 4 + q] for q in range(4)], writes=[b_xt[i]])

        load_xt(0)
        for gb in range(T // 512):
            seq_start = (gb % (SEQ // 512)) == 0
            if gb + 1 < T // 512:
                load_xt(gb + 1)
            x, bx = xt[gb % 2], b_xt[gb % 2]
            for h in range(8):
                hs = slice(h * 128, (h + 1) * 128)
                for (p, bp, off) in ((pq, b_pq, 0), (pf, b_pf, 1024), (pg, b_pg, 3072)):
                    for kc in range(8):
                        S.op("pe", lambda e: e.matmul(p[:], lhsT=w_in[:, kc, off + h * 128:off + (h + 1) * 128],
                                                      rhs=x[:, :, kc, :], start=(kc == 0), stop=(kc == 7)),
                             reads=[b_w, bx], writes=[bp])
                for c in range(8):
                    for kc in range(8):
                        S.op("pe", lambda e: e.matmul(pv[c // 4][0:64, c % 4, :], lhsT=x[:, c // 2, kc, (c % 2) * 64:(c % 2) * 64 + 64],
                                                      rhs=w_in[:, kc, 2048 + h * 128:2048 + (h + 1) * 128],
                                                      start=(kc == 0), stop=(kc == 7)),
                             reads=[b_w, bx], writes=[b_pv[c // 4]])
                S.op("act", lambda e: e.activation(out=f_[:], in_=pf[:], func=AF.Sigmoid), reads=[b_pf], writes=[b_f])
                S.op("dve", lambda e: e.tensor_scalar(out=f_[:], in0=f_[:], scalar1=sc[:, 3, h:h + 1],
                                                      scalar2=sc[:, 2, h:h + 1], op0=ALU.mult, op1=ALU.add),
                     reads=[b_f, b_sc], writes=[b_f])
                S.op("act", lambda e: e.activation(out=lf[:], in_=f_[:], func=AF.Ln), reads=[b_f], writes=[b_lf])
                S.op("dve", lambda e: e.tensor_tensor_scan(out=bb[:], data0=rmask[:].rearrange("p a b -> p (a b)"),
                                                           data1=lf[:], initial=0.0, op0=ALU.mult, op1=ALU.add),
                     reads=[b_lf, b_k], writes=[b_bb])
                S.op("act", lambda e: e.activation(out=eb[:], in_=bb[:], func=AF.Exp), reads=[b_bb], writes=[b_eb])
                S.op("act", lambda e: e.activation(out=enb[:], in_=bb[:], func=AF.Exp, scale=-1.0),
                     reads=[b_bb], writes=[b_enb])
                S.op("dve", lambda e: e.scalar_tensor_tensor(out=qn[:], in0=pq[:], scalar=-1.0, in1=eb[:],
                                                             op0=ALU.mult, op1=ALU.mult),
                     reads=[b_pq, b_eb], writes=[b_qn])
                S.op("dve", lambda e: e.scalar_tensor_tensor(out=kn[:], in0=f_[:], scalar=1.0, in1=enb[:],
                                                             op0=ALU.subtract, op1=ALU.mult),
                     reads=[b_f, b_enb], writes=[b_kn])
                S.op("act", lambda e: e.activation(out=sgt[:], in_=pg[:], func=AF.Silu), reads=[b_pg], writes=[b_sgt])
                for half in range(2):
                    S.op("act", lambda e: e.activation(out=v_sb[:, half * 4:(half + 1) * 4, :],
                                                       in_=pv[half][0:64, :, :], func=AF.Copy),
                         reads=[b_pv[half]], writes=[b_v])
                for c in range(8):
                    S.op("pe", lambda e: e.transpose(ptr[:, c, :], kn[:, c * 64:(c + 1) * 64], C.identb[:]),
                         reads=[b_kn, C.b_const], writes=[b_ptr])
                S.op("dve", lambda e: e.tensor_copy(out=kn_tok[:], in_=ptr[:]), reads=[b_ptr], writes=[b_kntok])
                for c in range(8):
                    cs = slice(c * 64, (c + 1) * 64)
                    k2 = c % 2
                    psc = pmisc[0:64, k2 * 64:(k2 + 1) * 64]
                    pkv = pmisc[:, 128:256]
                    first = seq_start and c == 0
                    S.op("pe", lambda e: e.matmul(psc, lhsT=kn[:, cs], rhs=qn[:, cs], start=True, stop=True),
                         reads=[b_kn, b_qn], writes=[b_psc[k2]])
                    S.op("dve", lambda e: e.tensor_tensor(out=scm[k2][:], in0=psc, in1=cmask[:], op=ALU.mult),
                         reads=[b_psc[k2], b_k], writes=[b_scm[k2]])
                    S.op("pe", lambda e: e.matmul(po[:, cs], lhsT=v_sb[:, c, :], rhs=scm[k2][:], start=True, stop=first),
                         reads=[b_v, b_scm[k2]], writes=[b_po])
                    if not first:
                        S.op("pe", lambda e: e.matmul(po[:, cs], lhsT=Sbf[:, h, :], rhs=qn[:, cs], start=False, stop=True),
                             reads=[b_Sbf[h], b_qn], writes=[b_po])
                    S.op("pe", lambda e: e.matmul(pkv, lhsT=kn_tok[:, c, :], rhs=v_sb[:, c, :], start=True, stop=True),
                         reads=[b_kntok, b_v], writes=[b_pkv])
                    ebl = eb[:, c * 64 + 63:c * 64 + 64]
                    if first:
                        S.op("dve", lambda e: e.tensor_scalar_mul(out=Sf[:, h, :], in0=pkv, scalar1=ebl),
                             reads=[b_pkv, b_eb], writes=[b_Sf[h]])
                    else:
                        S.op("dve", lambda e: e.tensor_scalar_mul(out=Sf[:, h, :], in0=Sf[:, h, :], scalar1=ebl),
                             reads=[b_eb, b_Sf[h]], writes=[b_Sf[h]])
                        S.op("dve", lambda e: e.scalar_tensor_tensor(out=Sf[:, h, :], in0=pkv, scalar=ebl,
                                                                     in1=Sf[:, h, :], op0=ALU.mult, op1=ALU.add),
                             reads=[b_pkv, b_eb, b_Sf[h]], writes=[b_Sf[h]])
                    S.op("act", lambda e: e.activation(out=Sbf[:, h, :], in_=Sf[:, h, :], func=AF.Copy),
                         reads=[b_Sf[h]], writes=[b_Sbf[h]])
                S.op("act", lambda e: e.activation(out=sq[:], in_=po[:], func=AF.Square), reads=[b_po], writes=[b_sq])
                S.op("pe", lambda e: e.matmul(pq[:], lhsT=ones[:], rhs=sq[:], start=True, stop=True),
                     reads=[b_sq, b_k], writes=[b_pq])
                S.op("act", lambda e: e.activation(out=rstd[:], in_=pq[:], func=AF.Ln, scale=1.0 / 128.0,
                                                   bias=epsb[:, 0:1]), reads=[b_pq, b_k], writes=[b_rstd])
                S.op("act", lambda e: e.activation(out=rstd[:], in_=rstd[:], func=AF.Exp, scale=-0.5),
                     reads=[b_rstd], writes=[b_rstd])
                S.op("dve", lambda e: e.tensor_tensor(out=on[:], in0=po[:], in1=rstd[:], op=ALU.mult),
                     reads=[b_po, b_rstd], writes=[b_on])
                S.op("dve", lambda e: e.scalar_tensor_tensor(out=onT[:, h, :], in0=on[:], scalar=sc[:, 4, h:h + 1],
                                                             in1=sgt[:], op0=ALU.mult, op1=ALU.mult),
                     reads=[b_on, b_sgt, b_sc], writes=[b_onT[h]])
            epi.prefetch(X_res, bX_res, gb * 4)
            for ts in range(4):
                tile = gb * 4 + ts
                if ts + 1 < 4:
                    epi.prefetch(X_res, bX_res, tile + 1)
                for nh, (p, bp) in enumerate(((pf, b_pf), (pg, b_pg))):
                    for h in range(8):
                        S.op("pe", lambda e: e.matmul(p[:], lhsT=onT[:, h, ts * 128:(ts + 1) * 128],
                                                      rhs=w_out[:, h, nh * 512:(nh + 1) * 512],
                                                      start=(h == 0), stop=(h == 7)),
                             reads=[b_onT[h], b_w], writes=[bp])
                    S.op("act", lambda e: e.activation(out=ymix[:, nh * 512:(nh + 1) * 512], in_=p[:], func=AF.Copy),
                         reads=[bp], writes=[b_ymix])
                epi.run(tile, ymix[:], [b_ymix], X_dst, bX_dst, XT_dst, bXT_dst)
        S.barrier()


TOPK = 256
IDX_SCALE = (8 ** -0.5) * (64 ** -0.5)
ATT_SCALE = 128 ** -0.5


def phase_dsa(S, nc, C, j, W, XT_src, bXT_src, X_res, bX_res, X_dst, bX_dst, XT_dst, bXT_dst,
              ln_g_row, ln_b_row, router, Gt, bG):
    with ExitStack() as st:
        sb = lambda name, shape, dt: st.enter_context(nc.sbuf_tensor(uq(name), shape, dt))
        ps = lambda name, shape, dt: st.enter_context(nc.psum_tensor(uq(name), shape, dt))
        win = sb("ds_win", [128, 8, 584], BF16)
        wuq = sb("ds_wuq", [128, 2, 1024], BF16)
        wqi = sb("ds_wqi", [128, 2, 512], BF16)
        wuk = sb("ds_wuk", [128, 8, 256], BF16)
        wuv = sb("ds_wuv", [128, 8, 2, 128], BF16)
        wout = sb("ds_wout", [128, 8, D], BF16)
        b_w = Buf("dsw")
        S.dma("pool", win[:], W["dsa_w_in"][j].rearrange("(kc p) n -> p kc n", p=128), writes=[b_w])
        S.dma("pool", wuq[:], W["dsa_w_uq"][j].rearrange("(kc p) n -> p kc n", p=128), writes=[b_w])
        S.dma("pool", wqi[:], W["dsa_w_qidx"][j].rearrange("(kc p) n -> p kc n", p=128), writes=[b_w])
        S.dma("pool", wuk[:], W["dsa_w_uk"][j].rearrange("h d c -> d h c"), writes=[b_w])
        S.dma("pool", wuv[:], W["dsa_w_uv"][j].rearrange("h (cc p) d -> p h cc d", p=128), writes=[b_w])
        S.dma("pool", wout[:], W["dsa_w_out"][j].rearrange("(kc p) n -> p kc n", p=128), writes=[b_w])
        gq = sb("ds_gq", [128, 256], F32)
        gkv = sb("ds_gkv", [128, 256], F32)
        kg = sb("ds_kg", [128, 64], F32)
        kb = sb("ds_kb", [128, 64], F32)
        b_g = Buf("dsg")
        S.dma("sp", gq[:], W["dsa_q_norm_g"][j].partition_broadcast(128), writes=[b_g])
        S.dma("sp", gkv[:], W["dsa_kv_norm_g"][j].partition_broadcast(128), writes=[b_g])
        S.dma("sp", kg[:], W["dsa_kidx_norm_g"][j].partition_broadcast(128), writes=[b_g])
        S.dma("sp", kb[:], W["dsa_kidx_norm_b"][j].partition_broadcast(128), writes=[b_g])
        onesb = sb("ds_ones", [128, 128], BF16)
        b_k = Buf("dsconst")
        S.op("pool", lambda e: e.memset(onesb[:], 1.0), writes=[b_k])
        ckvT = sb("ds_ckvT", [128, 2, SEQ], BF16)
        ckv = sb("ds_ckv", [128, NTS, 256], BF16)
        kidxT = sb("ds_kidxT", [64, SEQ], BF16)
        b_ckvT, b_ckv, b_kidxT = bufs(NTS, "ckvT"), bufs(NTS, "ckv"), bufs(NTS, "kidxT")
        xts = [sb("ds_xt%d" % i, [128, 8, 128], BF16) for i in range(2)]
        b_xts = bufs(2, "dsxt")
        cqT = sb("ds_cqT", [128, 2, 512], BF16)
        b_cqT = bufs(4, "cqT")
        qTh = [sb("ds_qTh%d" % i, [128, 512], BF16) for i in range(2)]
        b_qTh = bufs(2, "qTh")
        qlatT = sb("ds_qlatT", [128, 2, 8, 512], BF16)
        b_qlat = bufs(8, "qlat")
        qidxT = sb("ds_qidxT", [64, 8, 512], BF16)
        b_qidx = bufs(8, "qidx")
        widx = sb("ds_widx", [128, 4, 8], F32)
        b_widx = bufs(4, "widx")
        ss = sb("ds_ss", [128, 8], F32)
        b_ss = Buf("ss")
        junk = sb("ds_junk", [128, 256], BF16)
        b_junk = Buf("junk")
        cq = sb("ds_cq", [128, 256], BF16)
        b_cq = Buf("cq")
        kst = sb("ds_kst", [128, 16], F32)
        b_kst = Buf("kst")
        kx = sb("ds_kx", [128, 64], F32)
        kxb = sb("ds_kxb", [128, 64], BF16)
        b_kx = Buf("kx")
        acc = sb("ds_acc", [128, SEQ], F32)
        work = sb("ds_work", [128, SEQ], F32)
        b_acc, b_work = Buf("acc"), Buf("work")
        rr = [sb("ds_r%d" % i, [128, 512], F32) for i in range(2)]
        b_rr = bufs(2, "r")
        m8 = sb("ds_m8", [128, 8], F32)
        b_m8 = Buf("m8")
        masks = [sb("ds_mask%d" % i, [128, SEQ], BF16) for i in range(2)]
        b_masks = bufs(2, "mask")
        maskTs = [sb("ds_maskT%d" % i, [128, NTS, 128], BF16) for i in range(2)]
        b_maskTs = [bufs(NTS // 4, "maskTa"), bufs(NTS // 4, "maskTb")]
        E = [sb("ds_E%d" % i, [128, 4, 128], BF16) for i in range(2)]
        P = [sb("ds_P%d" % i, [128, 4, 128], BF16) for i in range(2)]
        b_E, b_P = bufs(2, "E"), bufs(2, "P")
        rden = sb("ds_rden", [128, 512], F32)
        b_rden = Buf("rden")
        posb = sb("ds_posb", [128, 4, 128], F32)
        b_posb = Buf("posb")
        olat = sb("ds_olat", [128, 2, 4, 128], BF16)
        b_olat = Buf("olat")
        oT = sb("ds_oT", [128, 8, 128], BF16)
        b_oT = bufs(2, "oT")
        pidx = [ps("ds_pidx%d" % i, [128, 512], F32) for i in range(2)]
        b_pidx = bufs(2, "pidx")
        pmt = ps("ds_pmt", [128, 8, 128], BF16)
        b_pmt = Buf("pmt")
        plg = [ps("ds_plg%d" % i, [128, 4, 128], F32) for i in range(2)]
        b_plg = bufs(2, "plg")
        polat = [ps("ds_pol%d" % i, [128, 4, 128], F32) for i in range(2)]
        b_pol = bufs(2, "pol")
        pden = ps("ds_pden", [128, 512], F32)
        b_pden = Buf("pden")
        epi = Epi(S, nc, st, C, ln_g_row, ln_b_row, router=router, pt=polat, b_pt=b_pol,
                  pr=pden[:, 0:NEXP], b_pr=b_pden, nbuf=1)

        for gb in range(T // 512):
            seq = gb // (SEQ // 512)
            blk = gb % (SEQ // 512)
            for ts in range(4):
                qi = blk * 4 + ts
                tcols = slice(ts * 128, (ts + 1) * 128)
                scols = slice(qi * 128, (qi + 1) * 128)
                xt, b_xt = xts[ts % 2], b_xts[ts % 2]
                S.dma("sp", xt[:], XT_src[gb * 4 + ts], reads=[bXT_src[gb * 4 + ts]], writes=[b_xt])
                pa0, bpa0 = plg[0], b_plg[0]
                pa1, bpa1 = plg[1], b_plg[1]
                pa0f = pa0[:].rearrange("p a b -> p (a b)")
                pa1f = pa1[:].rearrange("p a b -> p (a b)")
                for kc in range(8):
                    S.op("pe", lambda e: e.matmul(pa0f, lhsT=xt[:, kc, :], rhs=win[:, kc, 0:512],
                                                  start=(kc == 0), stop=(kc == 7)), reads=[b_xt, b_w], writes=[bpa0])
                for kc in range(8):
                    S.op("pe", lambda e: e.matmul(pa1f[:, 0:72], lhsT=xt[:, kc, :], rhs=win[:, kc, 512:584],
                                                  start=(kc == 0), stop=(kc == 7)), reads=[b_xt, b_w], writes=[bpa1])
                S.op("act", lambda e: e.activation(out=junk[:], in_=pa0f[:, 0:256], func=AF.Square,
                                                   accum_out=ss[:, 0:1]), reads=[bpa0], writes=[b_junk, b_ss])
                S.op("act", lambda e: e.activation(out=junk[:], in_=pa0f[:, 256:512], func=AF.Square,
                                                   accum_out=ss[:, 1:2]), reads=[bpa0], writes=[b_junk, b_ss])
                S.op("dve", lambda e: e.tensor_scalar(out=ss[:, 2:4], in0=ss[:, 0:2], scalar1=1.0 / 256.0,
                                                      scalar2=float(RMS_EPS), op0=ALU.mult, op1=ALU.add),
                     reads=[b_ss], writes=[b_ss])
                S.op("act", lambda e: e.activation(out=ss[:, 4:6], in_=ss[:, 2:4], func=AF.Ln), reads=[b_ss], writes=[b_ss])
                S.op("act", lambda e: e.activation(out=ss[:, 6:8], in_=ss[:, 4:6], func=AF.Exp, scale=-0.5),
                     reads=[b_ss], writes=[b_ss])
                S.op("dve", lambda e: e.scalar_tensor_tensor(out=cq[:], in0=pa0f[:, 0:256], scalar=ss[:, 6:7],
                                                             in1=gq[:], op0=ALU.mult, op1=ALU.mult),
                     reads=[bpa0, b_ss, b_g], writes=[b_cq])
                S.op("dve", lambda e: e.scalar_tensor_tensor(out=ckv[:, qi, :], in0=pa0f[:, 256:512], scalar=ss[:, 7:8],
                                                             in1=gkv[:], op0=ALU.mult, op1=ALU.mult),
                     reads=[bpa0, b_ss, b_g], writes=[b_ckv[qi]])
                S.op("dve", lambda e: e.bn_stats(out=kst[:, 0:6], in_=pa1f[:, 0:64]), reads=[bpa1], writes=[b_kst])
                S.op("dve", lambda e: e.bn_aggr(out=kst[:, 6:8], in_=kst[:, 0:6]), reads=[b_kst], writes=[b_kst])
                S.op("dve", lambda e: e.tensor_scalar_add(out=kst[:, 8:9], in0=kst[:, 7:8], scalar1=float(LN_EPS)),
                     reads=[b_kst], writes=[b_kst])
                S.op("act", lambda e: e.activation(out=kst[:, 9:10], in_=kst[:, 8:9], func=AF.Ln),
                     reads=[b_kst], writes=[b_kst])
                S.op("act", lambda e: e.activation(out=kst[:, 10:11], in_=kst[:, 9:10], func=AF.Exp, scale=-0.5),
                     reads=[b_kst], writes=[b_kst])
                S.op("dve", lambda e: e.tensor_scalar(out=kst[:, 11:12], in0=kst[:, 6:7], scalar1=-1.0,
                                                      scalar2=kst[:, 10:11], op0=ALU.mult, op1=ALU.mult),
                     reads=[b_kst], writes=[b_kst])
                S.op("act", lambda e: e.activation(out=kx[:], in_=pa1f[:, 0:64], func=AF.Identity,
                                                   scale=kst[:, 10:11], bias=kst[:, 11:12]),
                     reads=[bpa1, b_kst], writes=[b_kx])
                S.op("dve", lambda e: e.tensor_tensor(out=kx[:], in0=kx[:], in1=kg[:], op=ALU.mult),
                     reads=[b_kx, b_g], writes=[b_kx])
                S.op("dve", lambda e: e.tensor_tensor(out=kxb[:], in0=kx[:], in1=kb[:], op=ALU.add),
                     reads=[b_kx, b_g], writes=[b_kx])
                S.op("dve", lambda e: e.tensor_scalar_mul(out=widx[:, ts, :], in0=pa1f[:, 64:72], scalar1=float(IDX_SCALE)),
                     reads=[bpa1], writes=[b_widx[ts]])
                for cc in range(2):
                    S.op("pe", lambda e: e.transpose(pmt[:, cc, :], cq[:, cc * 128:(cc + 1) * 128], C.identb[:]),
                         reads=[b_cq, C.b_const], writes=[b_pmt])
                for cc in range(2):
                    S.op("pe", lambda e: e.transpose(pmt[:, 2 + cc, :], ckv[:, qi, cc * 128:(cc + 1) * 128], C.identb[:]),
                         reads=[b_ckv[qi], C.b_const], writes=[b_pmt])
                S.op("pe", lambda e: e.transpose(pmt[0:64, 4, :], kxb[:], C.identb[:]),
                     reads=[b_kx, C.b_const], writes=[b_pmt])
                S.op("act", lambda e: e.activation(out=cqT[:, :, tcols], in_=pmt[:, 0:2, :], func=AF.Copy),
                     reads=[b_pmt], writes=[b_cqT[ts]])
                S.op("act", lambda e: e.activation(out=ckvT[:, :, scols], in_=pmt[:, 2:4, :], func=AF.Copy),
                     reads=[b_pmt], writes=[b_ckvT[qi]])
                S.op("dve", lambda e: e.tensor_copy(out=kidxT[:, scols], in_=pmt[0:64, 4, :]),
                     reads=[b_pmt], writes=[b_kidxT[qi]])
            for h in range(8):
                pqh, bpqh = pidx[0], b_pidx[0]
                pqi, bpqi = pidx[1], b_pidx[1]
                qb, bqb = qTh[h % 2], b_qTh[h % 2]
                for cc in range(2):
                    S.op("pe", lambda e: e.matmul(pqh[:], lhsT=wuq[:, cc, h * 128:(h + 1) * 128], rhs=cqT[:, cc, :],
                                                  start=(cc == 0), stop=(cc == 1)), reads=[b_w] + b_cqT, writes=[bpqh])
                S.op("act", lambda e: e.activation(out=qb[:], in_=pqh[:], func=AF.Copy), reads=[bpqh], writes=[bqb])
                for cc in range(2):
                    pq_, bpq_ = polat[cc], b_pol[cc]
                    pq_f = pq_[:].rearrange("p a b -> p (a b)")
                    S.op("pe", lambda e: e.matmul(pq_f, lhsT=wuk[:, h, cc * 128:(cc + 1) * 128], rhs=qb[:],
                                                  start=True, stop=True), reads=[b_w, bqb], writes=[bpq_])
                    if cc == 0:
                        S.op("dve", lambda e: e.tensor_copy(out=qlatT[:, cc, h, :], in_=pq_f), reads=[bpq_],
                             writes=[b_qlat[h]])
                    else:
                        S.op("act", lambda e: e.activation(out=qlatT[:, cc, h, :], in_=pq_f, func=AF.Copy),
                             reads=[bpq_], writes=[b_qlat[h]])
                for cc in range(2):
                    S.op("pe", lambda e: e.matmul(pqi[0:64, :], lhsT=wqi[:, cc, h * 64:(h + 1) * 64], rhs=cqT[:, cc, :],
                                                  start=(cc == 0), stop=(cc == 1)), reads=[b_w] + b_cqT, writes=[bpqi])
                S.op("dve", lambda e: e.tensor_copy(out=qidxT[:, h, :], in_=pqi[0:64, :]), reads=[bpqi],
                     writes=[b_qidx[h]])
            def part_b1(ts):
                qi = blk * 4 + ts
                tile = gb * 4 + ts
                tcols = slice(ts * 128, (ts + 1) * 128)
                L = (qi + 1) * 128
                maskT, b_maskT = maskTs[ts % 2], b_maskTs[ts % 2]
                mask, b_mask = masks[ts % 2], b_masks[ts % 2]
                nseg = (L + 511) // 512
                for sg_ in range(nseg):
                    wd_ = min(512, L - sg_ * 512)
                    kcols = slice(sg_ * 512, sg_ * 512 + wd_)
                    kread = b_kidxT[sg_ * 4:sg_ * 4 + (wd_ // 128)]
                    for h in range(8):
                        k2 = (sg_ * 8 + h) % 2
                        S.op("pe", lambda e: e.matmul(pidx[k2][:, 0:wd_], lhsT=qidxT[:, h, tcols], rhs=kidxT[:, kcols],
                                                      start=True, stop=True),
                             reads=[b_qidx[h]] + kread, writes=[b_pidx[k2]])
                        S.op("act", lambda e: e.activation(out=rr[k2][:, 0:wd_], in_=pidx[k2][:, 0:wd_], func=AF.Relu),
                             reads=[b_pidx[k2]], writes=[b_rr[k2]])
                        if h == 0:
                            S.op("dve", lambda e: e.tensor_scalar_mul(out=acc[:, kcols], in0=rr[k2][:, 0:wd_],
                                                                      scalar1=widx[:, ts, 0:1]),
                                 reads=[b_rr[k2], b_widx[ts]], writes=[b_acc])
                        else:
                            S.op("dve", lambda e: e.scalar_tensor_tensor(out=acc[:, kcols], in0=rr[k2][:, 0:wd_],
                                                                         scalar=widx[:, ts, h:h + 1], in1=acc[:, kcols],
                                                                         op0=ALU.mult, op1=ALU.add),
                                 reads=[b_rr[k2], b_widx[ts], b_acc], writes=[b_acc])
                S.op("pool", lambda e: e.affine_select(out=acc[:, qi * 128:L], in_=acc[:, qi * 128:L], pattern=[[-1, 128]],
                                                       compare_op=ALU.is_ge, fill=C.negbig_reg, base=0,
                                                       channel_multiplier=1), reads=[b_acc], writes=[b_acc])
                if qi >= 2:
                    nr = TOPK // 8
                    for r in range(nr):
                        src = acc if r == 0 else work
                        bsrc = b_acc if r == 0 else b_work
                        S.op("dve", lambda e: e.max(out=m8[:], in_=src[:, 0:L]), reads=[bsrc], writes=[b_m8])
                        if r < nr - 1:
                            S.op("dve", lambda e: e.match_replace(out=work[:, 0:L], in_to_replace=m8[:],
                                                                  in_values=src[:, 0:L], imm_value=float(NEG_BIG)),
                                 reads=[bsrc, b_m8], writes=[b_work])
                    S.op("dve", lambda e: e.tensor_scalar(out=mask[:, 0:L], in0=acc[:, 0:L], scalar1=m8[:, 7:8],
                                                          scalar2=None, op0=ALU.is_ge), reads=[b_acc, b_m8], writes=[b_mask])
                else:
                    S.op("dve", lambda e: e.tensor_scalar(out=mask[:, 0:L], in0=acc[:, 0:L], scalar1=-1e29,
                                                          scalar2=None, op0=ALU.is_ge), reads=[b_acc], writes=[b_mask])

            def part_b2(ts):
                qi = blk * 4 + ts
                tile = gb * 4 + ts
                tcols = slice(ts * 128, (ts + 1) * 128)
                L = (qi + 1) * 128
                maskT, b_maskT = maskTs[ts % 2], b_maskTs[ts % 2]
                mask, b_mask = masks[ts % 2], b_masks[ts % 2]
                epi.prefetch(X_res, bX_res, tile)
                for g4 in range((qi + 4) // 4):
                    n4 = min(4, qi + 1 - g4 * 4)
                    for q4 in range(n4):
                        st_ = g4 * 4 + q4
                        S.op("pe", lambda e: e.transpose(pmt[:, q4, :], mask[:, st_ * 128:(st_ + 1) * 128], C.identb[:]),
                             reads=[b_mask, C.b_const], writes=[b_pmt])
                    S.op("act", lambda e: e.activation(out=maskT[:, g4 * 4:g4 * 4 + n4, :], in_=pmt[:, 0:n4, :],
                                                       func=AF.Copy), reads=[b_pmt], writes=[b_maskT[g4]])
                for hg in range(2):
                    for st_ in range(qi + 1):
                        k2 = st_ % 2
                        plf = plg[k2][:].rearrange("p a b -> p (a b)")
                        for cc in range(2):
                            S.op("pe", lambda e: e.matmul(plg[k2][:], lhsT=ckvT[:, cc, st_ * 128:(st_ + 1) * 128],
                                                          rhs=qlatT[:, cc, hg * 4:(hg + 1) * 4, tcols],
                                                          start=(cc == 0), stop=(cc == 1)),
                                 reads=[b_ckvT[st_]] + b_qlat[hg * 4:(hg + 1) * 4], writes=[b_plg[k2]])
                        S.op("act", lambda e: e.activation(out=E[k2][:], in_=plg[k2][:], func=AF.Exp, scale=float(ATT_SCALE)),
                             reads=[b_plg[k2]], writes=[b_E[k2]])
                        S.op("pool", lambda e: e.tensor_tensor(out=P[k2][:], in0=E[k2][:],
                                                               in1=maskT[:, st_, :].unsqueeze(1).broadcast_to([128, 4, 128]),
                                                               op=ALU.mult),
                             reads=[b_E[k2], b_maskT[st_ // 4]], writes=[b_P[k2]])
                        for cc in range(2):
                            S.op("pe", lambda e: e.matmul(polat[cc][:], lhsT=ckv[:, st_, cc * 128:(cc + 1) * 128], rhs=P[k2][:],
                                                          start=(st_ == 0), stop=(st_ == qi)),
                                 reads=[b_ckv[st_], b_P[k2]], writes=[b_pol[cc]])
                        S.op("pe", lambda e: e.matmul(pden[:], lhsT=onesb[:], rhs=P[k2][:].rearrange("p a b -> p (a b)"),
                                                      start=(st_ == 0), stop=(st_ == qi)),
                             reads=[b_k, b_P[k2]], writes=[b_pden])
                    S.op("act", lambda e: e.activation(out=rden[:], in_=pden[:], func=AF.Ln), reads=[b_pden], writes=[b_rden])
                    S.op("act", lambda e: e.activation(out=rden[:], in_=rden[:], func=AF.Exp, scale=-1.0),
                         reads=[b_rden], writes=[b_rden])
                    for cc in range(2):
                        S.op("act", lambda e: e.activation(out=olat[:, cc, :, :], in_=polat[cc][:], func=AF.Copy),
                             reads=[b_pol[cc]], writes=[b_olat])
                    po_, bpo_ = plg[0], b_plg[0]
                    for h4 in range(4):
                        h = hg * 4 + h4
                        for cc in range(2):
                            S.op("pe", lambda e: e.matmul(po_[:, h4, :], lhsT=wuv[:, h, cc, :], rhs=olat[:, cc, h4, :],
                                                          start=(cc == 0), stop=(cc == 1)),
                                 reads=[b_w, b_olat], writes=[bpo_])
                    S.op("act", lambda e: e.activation(out=posb[:], in_=po_[:], func=AF.Copy), reads=[bpo_], writes=[b_posb])
                    S.op("pool", lambda e: e.tensor_tensor(out=oT[:, hg * 4:(hg + 1) * 4, :], in0=posb[:],
                                                           in1=rden[:].rearrange("p (a b) -> p a b", a=4), op=ALU.mult),
                         reads=[b_posb, b_rden], writes=[b_oT[hg]])
                for nh in range(2):
                    for h in range(8):
                        S.op("pe", lambda e: e.matmul(pidx[nh][:], lhsT=oT[:, h, :], rhs=wout[:, h, nh * 512:(nh + 1) * 512],
                                                      start=(h == 0), stop=(h == 7)),
                             reads=[b_oT[h // 4], b_w], writes=[b_pidx[nh]])
                epi.run(tile, [pidx[0][:], pidx[1][:]], [b_pidx[0], b_pidx[1]], X_dst, bX_dst, XT_dst, bXT_dst,
                        gates_dst=Gt, bG=bG)

            part_b1(0)
            for ts in range(4):
                if ts + 1 < 4:
                    part_b1(ts + 1)
                part_b2(ts)
        S.barrier()


def emit_consts(S, nc, st, C):
    C.ident = st.enter_context(nc.sbuf_tensor("c_ident", [128, 128], F32))
    C.identb = st.enter_context(nc.sbuf_tensor("c_identb", [128, 128], BF16))
    C.b_const = Buf("const")
    C.negbig_reg = nc.gpsimd.to_reg(float(NEG_BIG))
    S.op("pool", lambda e: e.memset(C.ident[:], 0.0), writes=[C.b_const])
    S.op("pool", lambda e: e.affine_select(out=C.ident[:], in_=C.ident[:], pattern=[[-1, 128]],
                                           compare_op=ALU.not_equal, fill=1.0, base=0, channel_multiplier=1),
         reads=[C.b_const], writes=[C.b_const])
    S.op("pool", lambda e: e.tensor_copy(out=C.identb[:], in_=C.ident[:]), reads=[C.b_const], writes=[C.b_const])


WEIGHT_SPECS = [
    ("ln_g", [4, 2, 1024]), ("ln_b", [4, 2, 1024]),
    ("hg_w_in", [2, 1024, 4096]), ("hg_lower_bounds", [2, 1024]), ("hg_norm_g", [2, 8, 128]),
    ("hg_w_out", [2, 1024, 1024]),
    ("dsa_w_in", [2, 1024, 584]), ("dsa_q_norm_g", [2, 256]), ("dsa_kv_norm_g", [2, 256]),
    ("dsa_w_uq", [2, 256, 1024]), ("dsa_w_uk", [2, 8, 128, 256]), ("dsa_w_uv", [2, 8, 256, 128]),
    ("dsa_w_qidx", [2, 256, 512]), ("dsa_kidx_norm_g", [2, 64]), ("dsa_kidx_norm_b", [2, 64]),
    ("dsa_w_out", [2, 1024, 1024]),
    ("ffn_w_gate_up", [2, NFC, 128, 2 * 8 * FC]), ("ffn_w_down", [2, NFC, 128, (FC // 128) * 1024]),
    ("moe_w_router", [2, 128, 8 * 8]), ("moe_w_gate_up", [2, 8, NFC, 128, 2 * 8 * FC]),
    ("moe_w_down", [2, 8, NFC, 128, (FC // 128) * 1024]),
]


def build_program(plan=None):
    if plan is None:
        plan = list(range(8))
    nc = bass.Bass("TRN2", target_bir_lowering=False)
    x_in = nc.dram_tensor("x", [T, D], F32, kind="ExternalInput").ap()
    W = {name: nc.dram_tensor(name, shape, F32, kind="ExternalInput").ap() for name, shape in WEIGHT_SPECS}
    out = nc.dram_tensor("out", [T, D], F32, kind="ExternalOutput").ap()
    Xs = nc.dram_tensor("Xs", [T, D], F32, kind="Internal").ap()
    XT = [nc.dram_tensor("XT%d" % i, [NT, 128, 8, 128], BF16, kind="Internal").ap() for i in range(2)]
    Gt = nc.dram_tensor("Gt", [T, NEXP], F32, kind="Internal").ap()
    bX_in = bufs(NT, "xin")
    bXs = bufs(NT, "Xs")
    bXT = [bufs(NT, "XT0_"), bufs(NT, "XT1_")]
    bG = bufs(NT, "G")
    bOut = bufs(NT, "out")
    C = Ctx()
    with ExitStack() as st:
        S = Sched(nc, st)
        emit_consts(S, nc, st, C)
        cur_X, cur_bX = x_in, bX_in
        cur = 0
        phase_prep(S, nc, C, x_in, bX_in, XT[0], bXT[0])
        for si, sub in enumerate(plan):
            layer, kind = sub // 2, sub % 2
            j = layer // 2
            last = si == len(plan) - 1
            X_dst, bX_dst = (out, bOut) if last else (Xs, bXs)
            XT_dst, bXT_dst = (None, None) if last else (XT[1 - cur], bXT[1 - cur])
            if kind == 1:
                if layer % 2 == 0:
                    experts = [(W["ffn_w_gate_up"][j], W["ffn_w_down"][j])]
                    gates = None
                else:
                    experts = [(W["moe_w_gate_up"][j, e], W["moe_w_down"][j, e]) for e in range(NEXP)]
                    gates = Gt
                phase_ffn(S, nc, C, XT[cur], bXT[cur], cur_X, cur_bX, X_dst, bX_dst, XT_dst, bXT_dst,
                          experts, gates, bG, W["ln_g"][layer, 1], W["ln_b"][layer, 1])
            elif layer % 2 == 0:
                phase_hgrn(S, nc, C, j, W, XT[cur], bXT[cur], cur_X, cur_bX, X_dst, bX_dst, XT_dst, bXT_dst,
                           W["ln_g"][layer, 0], W["ln_b"][layer, 0])
            else:
                phase_dsa(S, nc, C, j, W, XT[cur], bXT[cur], cur_X, cur_bX, X_dst, bX_dst, XT_dst, bXT_dst,
                          W["ln_g"][layer, 0], W["ln_b"][layer, 0], W["moe_w_router"][j], Gt, bG)
            cur_X, cur_bX = X_dst, bX_dst
            cur = 1 - cur
        S.barrier()
        C.ninst = S.ninst
    return nc


def relayout_weights(inputs):
    out = {}
    for name, _ in WEIGHT_SPECS:
        w = np.asarray(inputs[name], dtype=np.float32)
        if name in ("ffn_w_gate_up", "moe_w_gate_up"):
            lead = w.shape[:-2]
            w = w.reshape(lead + (8, 128, 2, NFC, FC))
            nl = len(lead)
            perm = tuple(range(nl)) + (nl + 3, nl + 1, nl + 2, nl + 0, nl + 4)
            w = w.transpose(perm).reshape(lead + (NFC, 128, 2 * 8 * FC))
        elif name == "moe_w_router":
            w = w.reshape(2, 8, 128, NEXP).transpose(0, 2, 1, 3).reshape(2, 128, 8 * NEXP)
        elif name in ("ffn_w_down", "moe_w_down"):
            lead = w.shape[:-2]
            w = w.reshape(lead + (NFC, FC // 128, 128, D))
            nl = len(lead)
            perm = tuple(range(nl)) + (nl + 0, nl + 2, nl + 1, nl + 3)
            w = w.transpose(perm).reshape(lead + (NFC, 128, (FC // 128) * D))
        out[name] = np.ascontiguousarray(w)
    return out


_PROG = {}


def kernel(**inputs):
    x = np.ascontiguousarray(inputs["x"], dtype=np.float32)
    if "full" not in _PROG:
        _PROG["full"] = build_program()
    nc = _PROG["full"]
    wmap = relayout_weights(inputs)
    in_maps = []
    for c in range(8):
        m = dict(wmap)
        m["x"] = x[c * NSEQ:(c + 1) * NSEQ].reshape(T, D)
        in_maps.append(m)
    res = run_bass_kernel_spmd(nc, in_maps, core_ids=list(range(8)))
    outs = [np.asarray(r["out"]).reshape(NSEQ, SEQ, D) for r in res.results]
    return np.concatenate(outs, axis=0).astype(np.float32)
```

```python
from contextlib import ExitStack
import numpy as np
import concourse.bass as bass
import concourse.mybir as mybir
from concourse.bass_utils import run_bass_kernel_spmd

F32 = mybir.dt.float32
BF16 = mybir.dt.bfloat16
AF = mybir.ActivationFunctionType
ALU = mybir.AluOpType
AX = mybir.AxisListType

D = 1024
SEQ = 2048
NSEQ = 2
T = NSEQ * SEQ
NT = T // 128
NTS = SEQ // 128
DEPTH = 4
DFF = 3584
NEXP = 8
ALPHA = (2 * DEPTH) ** 0.25
LN_EPS = 1e-5
RMS_EPS = 1e-6
NEG_BIG = -1e30
FC = 512
NFC = DFF // FC

NDMA = 40
NDMA_SP = 28


class Buf:
    __slots__ = ("name", "w", "r")

    def __init__(self, name=""):
        self.name = name
        self.w = None
        self.r = {}


class Sched:
    def __init__(self, nc, stack):
        self.nc = nc
        self.engs = {"pe": nc.tensor, "act": nc.scalar, "dve": nc.vector,
                     "pool": nc.gpsimd, "sp": nc.sync}
        self.sem = {k: stack.enter_context(nc.semaphore("s_" + k)) for k in self.engs}
        self.cnt = {k: 0 for k in self.engs}
        self.seen = {k: {o: 0 for o in self.engs} for k in self.engs}
        self.dsem = [stack.enter_context(nc.semaphore("d%d" % i)) for i in range(NDMA)]
        self.dcnt = [0] * NDMA
        self.dseen = {k: [0] * NDMA for k in self.engs}
        self.rr = 0
        self.rrs = {}
        self.ninst = 0

    def _wait(self, en, tok):
        eng = self.engs[en]
        if tok[0] == "e":
            _, e2, idx = tok
            if e2 == en and en == "pe":
                return
            if self.seen[en][e2] >= idx:
                return
            eng.wait_ge(self.sem[e2], idx)
            self.seen[en][e2] = idx
        else:
            _, j, val = tok
            if self.dseen[en][j] >= val:
                return
            eng.wait_ge(self.dsem[j], val)
            self.dseen[en][j] = val

    def _deps(self, en, reads, writes):
        for b in reads:
            if b.w is not None:
                self._wait(en, b.w)
        for b in writes:
            if b.w is not None:
                self._wait(en, b.w)
            for t in b.r.values():
                self._wait(en, t)

    def _commit(self, tok, reads, writes):
        key = tok[1] if tok[0] == "e" else ("d", tok[1])
        for b in reads:
            b.r[key] = tok
        for b in writes:
            b.w = tok
            b.r = {}

    def op(self, en, fn, reads=(), writes=()):
        self._deps(en, reads, writes)
        ins = fn(self.engs[en])
        self.cnt[en] += 1
        ins.then_inc(self.sem[en], 1)
        self.ninst += 1
        self._commit(("e", en, self.cnt[en]), reads, writes)

    def dma(self, en, out, in_, reads=(), writes=(), **kw):
        lo, hi = (0, NDMA_SP) if en == "sp" else (NDMA_SP, NDMA)
        j = self.rrs.get(en, lo)
        self.rrs[en] = lo + (j + 1 - lo) % (hi - lo)
        self._deps(en, reads, writes)
        if self.dcnt[j] > 0:
            self._wait(en, ("d", j, self.dcnt[j]))
        ins = self.engs[en].dma_start(out=out, in_=in_, **kw)
        self.dcnt[j] += 16
        ins.then_inc(self.dsem[j], 16)
        self.ninst += 1
        self._commit(("d", j, self.dcnt[j]), reads, writes)

    def barrier(self):
        for en in self.engs:
            for e2 in self.engs:
                if self.cnt[e2] > 0:
                    self._wait(en, ("e", e2, self.cnt[e2]))
            for j in range(NDMA):
                if self.dcnt[j] > 0:
                    self._wait(en, ("d", j, self.dcnt[j]))


class Ctx:
    pass


_UID = [0]


def uq(name):
    _UID[0] += 1
    return "%s_u%d" % (name, _UID[0])


def bufs(n, name=""):
    return [Buf("%s%d" % (name, i)) for i in range(n)]


class Epi:
    def __init__(self, S, nc, st, C, ln_g_row, ln_b_row, router=None, pt=None, b_pt=None, pr=None, b_pr=None, nbuf=2):
        self.S, self.nc, self.C = S, nc, C
        sb = lambda name, shape, dt: st.enter_context(nc.sbuf_tensor(uq(name), shape, dt))
        self.nbuf = nbuf
        self.xres = [sb("ep_xres%d" % i, [128, D], F32) for i in range(nbuf)]
        self.b_xres = bufs(nbuf, "xres")
        self.gbc = sb("ep_g", [128, D], F32)
        self.bbc = sb("ep_b", [128, D], F32)
        self.b_gb = Buf("gb")
        self.stats = sb("ep_stats", [128, 12], F32)
        self.mv = sb("ep_mv", [128, 8], F32)
        self.b_small = Buf("small")
        self.xT = [sb("ep_xT%d" % i, [128, 8, 128], BF16) for i in range(nbuf)]
        self.b_xT = bufs(nbuf, "xT")
        if pt is None:
            self.pt = [st.enter_context(nc.psum_tensor(uq("ep_pt%d" % i), [128, 4, 128], F32)) for i in range(2)]
            self.b_pt = bufs(2, "ept")
        else:
            self.pt, self.b_pt = pt, b_pt
        self.k = 0
        self.pk = 0
        S.dma("sp", self.gbc[:], ln_g_row.partition_broadcast(128), writes=[self.b_gb])
        S.dma("sp", self.bbc[:], ln_b_row.partition_broadcast(128), writes=[self.b_gb])
        self.router = router
        if router is not None:
            self.wr = sb("ep_wr", [128, 8, NEXP], F32)
            self.b_wr = Buf("wr")
            S.dma("sp", self.wr[:], router.rearrange("p (kc e) -> p kc e", e=NEXP), writes=[self.b_wr])
            self.xTf = sb("ep_xTf", [128, 8, 128], F32)
            self.b_xTf = Buf("xTf")
            if pr is None:
                self.pr = st.enter_context(nc.psum_tensor(uq("ep_pr"), [128, NEXP], F32))[:]
                self.b_pr = Buf("pr")
            else:
                self.pr, self.b_pr = pr, b_pr
            self.rt = sb("ep_rt", [128, 6, NEXP], F32)
            self.b_rt = Buf("rt")

    def prefetch(self, X_src, bX_src, tile):
        i = self.pk % self.nbuf
        self.pk += 1
        self.S.dma("sp", self.xres[i][:], X_src[tile * 128:(tile + 1) * 128, :],
                   reads=[bX_src[tile]], writes=[self.b_xres[i]])

    def run(self, tile, y_ap, y_bufs, X_dst, bX_dst, XT_dst, bXT_dst, gates_dst=None, bG=None):
        S, C = self.S, self.C
        i = self.k % self.nbuf
        self.k += 1
        xr, bx = self.xres[i], self.b_xres[i]
        st_, mv, bs = self.stats, self.mv, self.b_small
        if isinstance(y_ap, (list, tuple)):
            for hh, (yh, yb) in enumerate(zip(y_ap, y_bufs)):
                S.op("dve", lambda e: e.scalar_tensor_tensor(out=xr[:, hh * 512:(hh + 1) * 512],
                                                             in0=xr[:, hh * 512:(hh + 1) * 512], scalar=float(ALPHA),
                                                             in1=yh, op0=ALU.mult, op1=ALU.add),
                     reads=[yb, bx], writes=[bx])
        else:
            S.op("dve", lambda e: e.scalar_tensor_tensor(out=xr[:], in0=xr[:], scalar=float(ALPHA), in1=y_ap,
                                                         op0=ALU.mult, op1=ALU.add),
                 reads=list(y_bufs) + [bx], writes=[bx])
        S.op("dve", lambda e: e.bn_stats(out=st_[:, 0:6], in_=xr[:, 0:512]), reads=[bx], writes=[bs])
        S.op("dve", lambda e: e.bn_stats(out=st_[:, 6:12], in_=xr[:, 512:1024]), reads=[bx], writes=[bs])
        S.op("dve", lambda e: e.bn_aggr(out=mv[:, 0:2], in_=st_[:, 0:12]), reads=[bs], writes=[bs])
        S.op("dve", lambda e: e.tensor_scalar_add(out=mv[:, 2:3], in0=mv[:, 1:2], scalar1=float(LN_EPS)),
             reads=[bs], writes=[bs])
        S.op("act", lambda e: e.activation(out=mv[:, 3:4], in_=mv[:, 2:3], func=AF.Ln), reads=[bs], writes=[bs])
        S.op("act", lambda e: e.activation(out=mv[:, 4:5], in_=mv[:, 3:4], func=AF.Exp, scale=-0.5),
             reads=[bs], writes=[bs])
        S.op("dve", lambda e: e.tensor_scalar(out=mv[:, 5:6], in0=mv[:, 0:1], scalar1=-1.0, scalar2=mv[:, 4:5],
                                              op0=ALU.mult, op1=ALU.mult), reads=[bs], writes=[bs])
        S.op("act", lambda e: e.activation(out=xr[:], in_=xr[:], func=AF.Identity, scale=mv[:, 4:5],
                                           bias=mv[:, 5:6]), reads=[bs, bx], writes=[bx])
        S.op("pool", lambda e: e.tensor_tensor(out=xr[:], in0=xr[:], in1=self.gbc[:], op=ALU.mult),
             reads=[bx, self.b_gb], writes=[bx])
        S.op("pool", lambda e: e.tensor_tensor(out=xr[:], in0=xr[:], in1=self.bbc[:], op=ALU.add),
             reads=[bx, self.b_gb], writes=[bx])
        if X_dst is not None:
            S.dma("sp", X_dst[tile * 128:(tile + 1) * 128, :], xr[:], reads=[bx], writes=[bX_dst[tile]])
        if XT_dst is None:
            return
        xT, bxT = self.xT[i], self.b_xT[i]
        for half in range(2):
            pt, bpt = self.pt[half], self.b_pt[half]
            for q in range(4):
                kc = half * 4 + q
                S.op("pe", lambda e: e.transpose(pt[:, q, :], xr[:, kc * 128:(kc + 1) * 128], C.ident[:]),
                     reads=[bx, C.b_const], writes=[bpt])
            if self.router is not None:
                S.op("act", lambda e: e.activation(out=self.xTf[:, half * 4:(half + 1) * 4, :], in_=pt[:], func=AF.Copy),
                     reads=[bpt], writes=[self.b_xTf])
            S.op("act", lambda e: e.activation(out=xT[:, half * 4:(half + 1) * 4, :], in_=pt[:], func=AF.Copy),
                 reads=[bpt], writes=[bxT])
        S.dma("sp", XT_dst[tile], xT[:], reads=[bxT], writes=[bXT_dst[tile]])
        if self.router is not None:
            rt, brt = self.rt, self.b_rt
            for kc in range(8):
                S.op("pe", lambda e: e.matmul(self.pr, lhsT=self.xTf[:, kc, :], rhs=self.wr[:, kc, :],
                                              start=(kc == 0), stop=(kc == 7)),
                     reads=[self.b_xTf, self.b_wr], writes=[self.b_pr])
            S.op("dve", lambda e: e.tensor_copy(out=rt[:, 0, :], in_=self.pr), reads=[self.b_pr], writes=[brt])
            S.op("dve", lambda e: e.max(out=rt[:, 1, :], in_=rt[:, 0, :]), reads=[brt], writes=[brt])
            S.op("dve", lambda e: e.tensor_scalar_mul(out=rt[:, 2, 0:1], in0=rt[:, 1, 0:1], scalar1=-1.0),
                 reads=[brt], writes=[brt])
            S.op("act", lambda e: e.activation(out=rt[:, 3, :], in_=rt[:, 0, :], func=AF.Exp, bias=rt[:, 2, 0:1],
                                               scale=1.0), reads=[brt], writes=[brt])
            S.op("dve", lambda e: e.scalar_tensor_tensor(out=rt[:, 4, :], in0=rt[:, 0, :], scalar=rt[:, 1, 1:2],
                                                         in1=rt[:, 3, :], op0=ALU.is_ge, op1=ALU.mult),
                 reads=[brt], writes=[brt])
            S.op("dve", lambda e: e.reduce_sum(out=rt[:, 2, 1:2], in_=rt[:, 4, :], axis=AX.X),
                 reads=[brt], writes=[brt])
            S.op("dve", lambda e: e.reciprocal(out=rt[:, 2, 2:3], in_=rt[:, 2, 1:2]), reads=[brt], writes=[brt])
            S.op("dve", lambda e: e.tensor_scalar_mul(out=rt[:, 5, :], in0=rt[:, 4, :], scalar1=rt[:, 2, 2:3]),
                 reads=[brt], writes=[brt])
            S.dma("sp", gates_dst[tile * 128:(tile + 1) * 128, :], rt[:, 5, :], reads=[brt], writes=[bG[tile]])


def phase_prep(S, nc, C, X_src, bX_src, XT_dst, bXT_dst):
    with ExitStack() as st:
        sb = lambda name, shape, dt: st.enter_context(nc.sbuf_tensor(uq(name), shape, dt))
        xin = [sb("pp_x%d" % i, [128, D], F32) for i in range(2)]
        b_xin = bufs(2)
        xT = [sb("pp_xT%d" % i, [128, 8, 128], BF16) for i in range(2)]
        b_xT = bufs(2)
        pt = [st.enter_context(nc.psum_tensor(uq("pp_pt%d" % i), [128, 4, 128], F32)) for i in range(2)]
        b_pt = bufs(2)
        for tile in range(NT):
            i = tile % 2
            S.dma("sp", xin[i][:], X_src[tile * 128:(tile + 1) * 128, :], reads=[bX_src[tile]], writes=[b_xin[i]])
            for half in range(2):
                for q in range(4):
                    kc = half * 4 + q
                    S.op("pe", lambda e: e.transpose(pt[half][:, q, :], xin[i][:, kc * 128:(kc + 1) * 128],
                                                     C.ident[:]),
                         reads=[b_xin[i], C.b_const], writes=[b_pt[half]])
                eng = "act" if half == 0 else "dve"
                if eng == "act":
                    S.op("act", lambda e: e.activation(out=xT[i][:, half * 4:(half + 1) * 4, :], in_=pt[half][:],
                                                       func=AF.Copy), reads=[b_pt[half]], writes=[b_xT[i]])
                else:
                    S.op("dve", lambda e: e.tensor_copy(out=xT[i][:, half * 4:(half + 1) * 4, :], in_=pt[half][:]),
                         reads=[b_pt[half]], writes=[b_xT[i]])
            S.dma("sp", XT_dst[tile], xT[i][:], reads=[b_xT[i]], writes=[bXT_dst[tile]])
        S.barrier()


def phase_ffn(S, nc, C, XT_src, bXT_src, X_res, bX_res, X_dst, bX_dst, XT_dst, bXT_dst,
              experts, gates, bG, ln_g_row, ln_b_row):
    moe = gates is not None
    with ExitStack() as st:
        sb = lambda name, shape, dt: st.enter_context(nc.sbuf_tensor(uq(name), shape, dt))
        yacc = sb("ff_y", [128, NTS, D], F32)
        b_y = bufs(NTS, "y")
        xt = sb("ff_xt", [128, NTS, 8, 128], BF16)
        b_xt = bufs(4, "xt")
        NW = 2
        wgu = [sb("ff_wgu%d" % i, [128, 2, 8, FC], BF16) for i in range(NW)]
        wg = [w_[:, 0, :, :] for w_ in wgu]
        wu = [w_[:, 1, :, :] for w_ in wgu]
        wd = [sb("ff_wd%d" % i, [128, FC // 128, D], BF16) for i in range(NW)]
        b_w = bufs(NW, "w")
        NFS = FC // 128
        hT = [sb("ff_h%d" % i, [128, NFS, 512], BF16) for i in range(2)]
        b_h = bufs(2, "h")
        sg = [sb("ff_s%d" % i, [128, 512], BF16) for i in range(2)]
        b_s = bufs(2, "s")
        if moe:
            gt = sb("ff_gt", [128, NTS, NEXP], F32)
            b_gt = Buf("gt")
        pg = [st.enter_context(nc.psum_tensor(uq("ff_pg%d" % i), [128, 512], F32)) for i in range(2)]
        pu = [st.enter_context(nc.psum_tensor(uq("ff_pu%d" % i), [128, 512], F32)) for i in range(2)]
        b_pg, b_pu = bufs(2, "pg"), bufs(2, "pu")
        py = [st.enter_context(nc.psum_tensor(uq("ff_py%d" % i), [128, 512], F32)) for i in range(2)]
        b_py = bufs(2, "py")
        epi = Epi(S, nc, st, C, ln_g_row, ln_b_row)

        for seq in range(NSEQ):
            t0 = seq * SEQ
            for blk in range(4):
                S.dma("sp", xt[:, blk * 4:(blk + 1) * 4, :, :],
                      XT_src[t0 // 128 + blk * 4:t0 // 128 + (blk + 1) * 4].rearrange("n p kc t -> p n kc t"),
                      reads=[bXT_src[(t0 + blk * 512) // 128 + q] for q in range(4)], writes=[b_xt[blk]])
            if moe:
                for q in range(NTS):
                    S.dma("sp", gt[:, q, :], gates[t0 + q * 128:t0 + (q + 1) * 128, :],
                          reads=[bG[t0 // 128 + q]], writes=[b_gt])
            chunks = [(e, fc) for e in range(len(experts)) for fc in range(NFC)]
            items = [(ci, tb) for ci in range(len(chunks)) for tb in range(4)]

            def load_w(ci):
                e, fc = chunks[ci]
                w_gu, w_d = experts[e]
                sl = ci % NW
                S.dma("pool", wgu[sl][:].rearrange("p a k f -> p (a k f)").rearrange("p (c f) -> p c f", f=2048),
                      w_gu[fc].rearrange("p (c f) -> p c f", f=2048), writes=[b_w[sl]])
                S.dma("pool", wd[sl][:].rearrange("p a n -> p (a n)").rearrange("p (c f) -> p c f", f=2048),
                      w_d[fc].rearrange("p (c f) -> p c f", f=2048), writes=[b_w[sl]])

            def up(k):
                ci, tb = items[k]
                sl = ci % NW
                h, bh = hT[k % 2], b_h[k % 2]
                for fs in range(NFS):
                    j = (k * NFS + fs) % 2
                    for kc in range(8):
                        S.op("pe", lambda e: e.matmul(pg[j][:], lhsT=wg[sl][:, kc, fs * 128:(fs + 1) * 128],
                                                      rhs=xt[:, tb * 4:(tb + 1) * 4, kc, :],
                                                      start=(kc == 0), stop=(kc == 7)),
                             reads=[b_w[sl], b_xt[tb]], writes=[b_pg[j]])
                    for kc in range(8):
                        S.op("pe", lambda e: e.matmul(pu[j][:], lhsT=wu[sl][:, kc, fs * 128:(fs + 1) * 128],
                                                      rhs=xt[:, tb * 4:(tb + 1) * 4, kc, :],
                                                      start=(kc == 0), stop=(kc == 7)),
                             reads=[b_w[sl], b_xt[tb]], writes=[b_pu[j]])
                    S.op("act", lambda e: e.activation(out=sg[j][:], in_=pg[j][:], func=AF.Silu),
                         reads=[b_pg[j]], writes=[b_s[j]])
                    S.op("dve", lambda e: e.tensor_tensor(out=h[:, fs, :], in0=sg[j][:], in1=pu[j][:], op=ALU.mult),
                         reads=[b_s[j], b_pu[j]], writes=[bh])

            def down(k):
                ci, tb = items[k]
                e_idx, fc = chunks[ci]
                sl = ci % NW
                h, bh = hT[k % 2], b_h[k % 2]
                for ts in range(4):
                    tl = tb * 4 + ts
                    for nh in range(2):
                        j = (ts * 2 + nh) % 2
                        for fs in range(NFS):
                            S.op("pe", lambda e: e.matmul(py[j][:], lhsT=h[:, fs, ts * 128:(ts + 1) * 128],
                                                          rhs=wd[sl][:, fs, nh * 512:(nh + 1) * 512],
                                                          start=(fs == 0), stop=(fs == NFS - 1)),
                                 reads=[bh, b_w[sl]], writes=[b_py[j]])
                        ya = yacc[:, tl, nh * 512:(nh + 1) * 512]
                        if moe:
                            gsc = gt[:, tl, e_idx:e_idx + 1]
                            if ci == 0:
                                S.op("dve", lambda e: e.tensor_scalar_mul(out=ya, in0=py[j][:], scalar1=gsc),
                                     reads=[b_py[j], b_gt], writes=[b_y[tl]])
                            else:
                                S.op("dve", lambda e: e.scalar_tensor_tensor(out=ya, in0=py[j][:], scalar=gsc, in1=ya,
                                                                             op0=ALU.mult, op1=ALU.add),
                                     reads=[b_py[j], b_gt, b_y[tl]], writes=[b_y[tl]])
                        else:
                            if ci == 0:
                                S.op("dve", lambda e: e.tensor_copy(out=ya, in_=py[j][:]),
                                     reads=[b_py[j]], writes=[b_y[tl]])
                            else:
                                S.op("dve", lambda e: e.tensor_tensor(out=ya, in0=py[j][:], in1=ya, op=ALU.add),
                                     reads=[b_py[j], b_y[tl]], writes=[b_y[tl]])

            load_w(0)
            for k in range(len(items)):
                ci, tb = items[k]
                if tb == 0 and ci + 1 < len(chunks):
                    load_w(ci + 1)
                if k == 0:
                    up(0)
                if k + 1 < len(items):
                    up(k + 1)
                down(k)
            epi.prefetch(X_res, bX_res, t0 // 128)
            for tl in range(NTS):
                tile = t0 // 128 + tl
                if tl + 1 < NTS:
                    epi.prefetch(X_res, bX_res, tile + 1)
                epi.run(tile, yacc[:, tl, :], [b_y[tl]], X_dst, bX_dst, XT_dst, bXT_dst)
        S.barrier()


def phase_hgrn(S, nc, C, j, W, XT_src, bXT_src, X_res, bX_res, X_dst, bX_dst, XT_dst, bXT_dst,
               ln_g_row, ln_b_row):
    w_in_d, w_out_d = W["hg_w_in"][j], W["hg_w_out"][j]
    with ExitStack() as st:
        sb = lambda name, shape, dt: st.enter_context(nc.sbuf_tensor(uq(name), shape, dt))
        ps = lambda name, shape, dt: st.enter_context(nc.psum_tensor(uq(name), shape, dt))
        w_in = sb("hg_win", [128, 8, 4096], BF16)
        w_out = sb("hg_wout", [128, 8, D], BF16)
        b_w = Buf("hgw")
        for sec in range(4):
            S.dma("pool", w_in[:, :, sec * 1024:(sec + 1) * 1024],
                  w_in_d[:, sec * 1024:(sec + 1) * 1024].rearrange("(kc p) n -> p kc n", p=128), writes=[b_w])
        S.dma("pool", w_out[:], w_out_d.rearrange("(kc p) n -> p kc n", p=128), writes=[b_w])
        sc = sb("hg_sc", [128, 5, 8], F32)
        b_sc = Buf("hgsc")
        S.dma("sp", sc[:, 4, :], W["hg_norm_g"][j].rearrange("h p -> p h"), writes=[b_sc],
              allow_slow_non_contiguous=True)
        if j == 0:
            S.op("dve", lambda e: e.memset(sc[:, 2, :], 0.0), writes=[b_sc])
            S.op("dve", lambda e: e.memset(sc[:, 3, :], 1.0), writes=[b_sc])
        else:
            S.dma("sp", sc[:, 0, :], W["hg_lower_bounds"][0].rearrange("(h p) -> p h", p=128), writes=[b_sc],
                  allow_slow_non_contiguous=True)
            S.dma("sp", sc[:, 1, :], W["hg_lower_bounds"][1].rearrange("(h p) -> p h", p=128), writes=[b_sc],
                  allow_slow_non_contiguous=True)
            S.op("dve", lambda e: e.tensor_tensor(out=sc[:, 1, :], in0=sc[:, 1, :], in1=sc[:, 0, :], op=ALU.subtract),
                 reads=[b_sc], writes=[b_sc])
            S.op("act", lambda e: e.activation(out=sc[:, 2, :], in_=sc[:, 1, :], func=AF.Sigmoid),
                 reads=[b_sc], writes=[b_sc])
            S.op("dve", lambda e: e.tensor_scalar(out=sc[:, 3, :], in0=sc[:, 2, :], scalar1=-1.0, scalar2=1.0,
                                                  op0=ALU.mult, op1=ALU.add), reads=[b_sc], writes=[b_sc])
        rmask = sb("hg_rmask", [128, 8, 64], F32)
        cmask = sb("hg_cmask", [64, 64], F32)
        ones = sb("hg_ones", [128, 128], F32)
        epsb = sb("hg_eps", [128, 1], F32)
        b_k = Buf("hgconst")
        S.op("pool", lambda e: e.memset(rmask[:], 1.0), writes=[b_k])
        S.op("pool", lambda e: e.memset(rmask[:, :, 0:1], 0.0), reads=[b_k], writes=[b_k])
        S.op("pool", lambda e: e.memset(cmask[:], 1.0), reads=[b_k], writes=[b_k])
        S.op("pool", lambda e: e.affine_select(out=cmask[:], in_=cmask[:], pattern=[[1, 64]], compare_op=ALU.is_ge,
                                               fill=0.0, base=0, channel_multiplier=-1), reads=[b_k], writes=[b_k])
        S.op("pool", lambda e: e.memset(ones[:], 1.0), reads=[b_k], writes=[b_k])
        S.op("pool", lambda e: e.memset(epsb[:], float(RMS_EPS)), reads=[b_k], writes=[b_k])
        xt = [sb("hg_xt%d" % i, [128, 4, 8, 128], BF16) for i in range(2)]
        b_xt = bufs(2, "hgxt")
        f_ = sb("hg_f", [128, 512], F32)
        lf = sb("hg_lf", [128, 512], F32)
        bb = sb("hg_b", [128, 512], F32)
        ebs = [sb("hg_eb%d" % i, [128, 512], F32) for i in range(2)]
        enb = sb("hg_enb", [128, 512], F32)
        b_f, b_lf, b_bb, b_enb = [Buf(n) for n in ("f", "lf", "bb", "enb")]
        b_ebs = bufs(2, "eb")
        qns = [sb("hg_qn%d" % i, [128, 512], BF16) for i in range(2)]
        kns = [sb("hg_kn%d" % i, [128, 512], BF16) for i in range(2)]
        sgts = [sb("hg_sgt%d" % i, [128, 512], BF16) for i in range(2)]
        b_qns, b_kns, b_sgts = bufs(2, "qn"), bufs(2, "kn"), bufs(2, "sgt")
        v_sbs = [sb("hg_v%d" % i, [64, 8, 128], BF16) for i in range(2)]
        kn_toks = [sb("hg_kntok%d" % i, [64, 8, 128], BF16) for i in range(2)]
        b_vs, b_kntoks = bufs(2, "v"), bufs(2, "kntok")
        scm8 = sb("hg_scm8", [64, 8, 64], BF16)
        b_scm8 = bufs(8, "scm")
        Sf = sb("hg_Sf", [128, 8, 128], F32)
        Sbf = sb("hg_Sbf", [128, 8, 128], BF16)
        b_Sf, b_Sbf = bufs(8, "Sf"), bufs(8, "Sbf")
        sq = sb("hg_sq", [128, 512], F32)
        rstd = sb("hg_rstd", [128, 512], F32)
        on = sb("hg_on", [128, 512], F32)
        b_sq, b_rstd, b_on = Buf("sq"), Buf("rstd"), Buf("on")
        onT = sb("hg_onT", [128, 8, 512], BF16)
        b_onT = bufs(8, "onT")
        ymix = sb("hg_ymix", [128, D], F32)
        b_ymix = Buf("ymix")
        pq, pf, pg = ps("hg_pq", [128, 512], F32), ps("hg_pf", [128, 512], F32), ps("hg_pg", [128, 512], F32)
        b_pq, b_pf, b_pg = Buf("pq"), Buf("pf"), Buf("pg")
        pv = [ps("hg_pv%d" % i, [128, 4, 128], F32) for i in range(2)]
        b_pv = bufs(2, "pv")
        po = ps("hg_po", [128, 512], F32)
        b_po = Buf("po")
        pmisc = ps("hg_pmisc", [128, 512], F32)
        b_psc8 = bufs(8, "psc")
        ptr = ps("hg_ptr", [64, 8, 128], BF16)
        b_ptr = Buf("ptr")
        epi = Epi(S, nc, st, C, ln_g_row, ln_b_row, pt=pv, b_pt=b_pv)

        def load_xt(gb):
            i = gb % 2
            S.dma("sp", xt[i][:], XT_src[gb * 4:(gb + 1) * 4].rearrange("n p kc t -> p n kc t"),
                  reads=[bXT_src[gb * 4 + q] for q in range(4)], writes=[b_xt[i]])

        load_xt(0)
        for gb in range(T // 512):
            seq_start = (gb % (SEQ // 512)) == 0
            if gb + 1 < T // 512:
                load_xt(gb + 1)
            x, bx = xt[gb % 2], b_xt[gb % 2]
            def part_a(h):
                for (p, bp, off) in ((pq, b_pq, 0), (pf, b_pf, 1024), (pg, b_pg, 3072)):
                    for kc in range(8):
                        S.op("pe", lambda e: e.matmul(p[:], lhsT=w_in[:, kc, off + h * 128:off + (h + 1) * 128],
                                                      rhs=x[:, :, kc, :], start=(kc == 0), stop=(kc == 7)),
                             reads=[b_w, bx], writes=[bp])
                for c in range(8):
                    for kc in range(8):
                        S.op("pe", lambda e: e.matmul(pv[c // 4][0:64, c % 4, :], lhsT=x[:, c // 2, kc, (c % 2) * 64:(c % 2) * 64 + 64],
                                                      rhs=w_in[:, kc, 2048 + h * 128:2048 + (h + 1) * 128],
                                                      start=(kc == 0), stop=(kc == 7)),
                             reads=[b_w, bx], writes=[b_pv[c // 4]])

            def part_b(h):
                hp = h % 2
                qn, b_qn, kn, b_kn, sgt, b_sgt = qns[hp], b_qns[hp], kns[hp], b_kns[hp], sgts[hp], b_sgts[hp]
                v_sb, b_v, kn_tok, b_kntok, eb, b_eb = v_sbs[hp], b_vs[hp], kn_toks[hp], b_kntoks[hp], ebs[hp], b_ebs[hp]
                S.op("act", lambda e: e.activation(out=f_[:], in_=pf[:], func=AF.Sigmoid), reads=[b_pf], writes=[b_f])
                S.op("dve", lambda e: e.tensor_scalar(out=f_[:], in0=f_[:], scalar1=sc[:, 3, h:h + 1],
                                                      scalar2=sc[:, 2, h:h + 1], op0=ALU.mult, op1=ALU.add),
                     reads=[b_f, b_sc], writes=[b_f])
                S.op("act", lambda e: e.activation(out=lf[:], in_=f_[:], func=AF.Ln), reads=[b_f], writes=[b_lf])
                S.op("dve", lambda e: e.tensor_tensor_scan(out=bb[:], data0=rmask[:].rearrange("p a b -> p (a b)"),
                                                           data1=lf[:], initial=0.0, op0=ALU.mult, op1=ALU.add),
                     reads=[b_lf, b_k], writes=[b_bb])
                S.op("act", lambda e: e.activation(out=eb[:], in_=bb[:], func=AF.Exp), reads=[b_bb], writes=[b_eb])
                S.op("act", lambda e: e.activation(out=enb[:], in_=bb[:], func=AF.Exp, scale=-1.0),
                     reads=[b_bb], writes=[b_enb])
                S.op("dve", lambda e: e.scalar_tensor_tensor(out=qn[:], in0=pq[:], scalar=-1.0, in1=eb[:],
                                                             op0=ALU.mult, op1=ALU.mult),
                     reads=[b_pq, b_eb], writes=[b_qn])
                S.op("dve", lambda e: e.scalar_tensor_tensor(out=kn[:], in0=f_[:], scalar=1.0, in1=enb[:],
                                                             op0=ALU.subtract, op1=ALU.mult),
                     reads=[b_f, b_enb], writes=[b_kn])
                S.op("act", lambda e: e.activation(out=sgt[:], in_=pg[:], func=AF.Silu), reads=[b_pg], writes=[b_sgt])
                for half in range(2):
                    S.op("act", lambda e: e.activation(out=v_sb[:, half * 4:(half + 1) * 4, :],
                                                       in_=pv[half][0:64, :, :], func=AF.Copy),
                         reads=[b_pv[half]], writes=[b_v])
                for c in range(8):
                    S.op("pe", lambda e: e.transpose(ptr[:, c, :], kn[:, c * 64:(c + 1) * 64], C.identb[:]),
                         reads=[b_kn, C.b_const], writes=[b_ptr])
                S.op("dve", lambda e: e.tensor_copy(out=kn_tok[:], in_=ptr[:]), reads=[b_ptr], writes=[b_kntok])

            def part_cd(h):
                hp = h % 2
                qn, b_qn, kn, b_kn, sgt, b_sgt = qns[hp], b_qns[hp], kns[hp], b_kns[hp], sgts[hp], b_sgts[hp]
                v_sb, b_v, kn_tok, b_kntok, eb, b_eb = v_sbs[hp], b_vs[hp], kn_toks[hp], b_kntoks[hp], ebs[hp], b_ebs[hp]
                for c in range(8):
                    cs = slice(c * 64, (c + 1) * 64)
                    psc = pmisc[0:64, cs]
                    S.op("pe", lambda e: e.matmul(psc, lhsT=kn[:, cs], rhs=qn[:, cs], start=True, stop=True),
                         reads=[b_kn, b_qn], writes=[b_psc8[c]])
                    S.op("dve", lambda e: e.tensor_tensor(out=scm8[:, c, :], in0=psc, in1=cmask[:], op=ALU.mult),
                         reads=[b_psc8[c], b_k], writes=[b_scm8[c]])
                for c in range(8):
                    S.op("pe", lambda e: e.matmul(pv[c // 4][:, c % 4, :], lhsT=kn_tok[:, c, :], rhs=v_sb[:, c, :],
                                                  start=True, stop=True),
                         reads=[b_kntok, b_v], writes=[b_pv[c // 4]])
                for c in range(8):
                    cs = slice(c * 64, (c + 1) * 64)
                    pkv = pv[c // 4][:, c % 4, :]
                    b_pkv = b_pv[c // 4]
                    first = seq_start and c == 0
                    S.op("pe", lambda e: e.matmul(po[:, cs], lhsT=v_sb[:, c, :], rhs=scm8[:, c, :], start=True, stop=first),
                         reads=[b_v, b_scm8[c]], writes=[b_po])
                    if not first:
                        S.op("pe", lambda e: e.matmul(po[:, cs], lhsT=Sbf[:, h, :], rhs=qn[:, cs], start=False, stop=True),
                             reads=[b_Sbf[h], b_qn], writes=[b_po])
                    ebl = eb[:, c * 64 + 63:c * 64 + 64]
                    if first:
                        S.op("dve", lambda e: e.tensor_scalar_mul(out=Sf[:, h, :], in0=pkv, scalar1=ebl),
                             reads=[b_pkv, b_eb], writes=[b_Sf[h]])
                    else:
                        S.op("dve", lambda e: e.tensor_scalar_mul(out=Sf[:, h, :], in0=Sf[:, h, :], scalar1=ebl),
                             reads=[b_eb, b_Sf[h]], writes=[b_Sf[h]])
                        S.op("dve", lambda e: e.scalar_tensor_tensor(out=Sf[:, h, :], in0=pkv, scalar=ebl,
                                                                     in1=Sf[:, h, :], op0=ALU.mult, op1=ALU.add),
                             reads=[b_pkv, b_eb, b_Sf[h]], writes=[b_Sf[h]])
                    if c < 7 or True:
                        S.op("act", lambda e: e.activation(out=Sbf[:, h, :], in_=Sf[:, h, :], func=AF.Copy),
                             reads=[b_Sf[h]], writes=[b_Sbf[h]])
                S.op("act", lambda e: e.activation(out=sq[:], in_=po[:], func=AF.Square), reads=[b_po], writes=[b_sq])
                S.op("pe", lambda e: e.matmul(pmisc[:], lhsT=ones[:], rhs=sq[:], start=True, stop=True),
                     reads=[b_sq, b_k], writes=b_psc8)
                S.op("act", lambda e: e.activation(out=rstd[:], in_=pmisc[:], func=AF.Ln, scale=1.0 / 128.0,
                                                   bias=epsb[:, 0:1]), reads=b_psc8 + [b_k], writes=[b_rstd])
                S.op("act", lambda e: e.activation(out=rstd[:], in_=rstd[:], func=AF.Exp, scale=-0.5),
                     reads=[b_rstd], writes=[b_rstd])
                S.op("dve", lambda e: e.tensor_tensor(out=on[:], in0=po[:], in1=rstd[:], op=ALU.mult),
                     reads=[b_po, b_rstd], writes=[b_on])
                S.op("dve", lambda e: e.scalar_tensor_tensor(out=onT[:, h, :], in0=on[:], scalar=sc[:, 4, h:h + 1],
                                                             in1=sgt[:], op0=ALU.mult, op1=ALU.mult),
                     reads=[b_on, b_sgt, b_sc], writes=[b_onT[h]])

            part_a(0)
            part_b(0)
            for h in range(8):
                if h + 1 < 8:
                    part_a(h + 1)
                    part_b(h + 1)
                part_cd(h)
            epi.prefetch(X_res, bX_res, gb * 4)
            for ts in range(4):
                tile = gb * 4 + ts
                if ts + 1 < 4:
                    epi.prefetch(X_res, bX_res, tile + 1)
                for nh, (p, bp) in enumerate(((pf, b_pf), (pg, b_pg))):
                    for h in range(8):
                        S.op("pe", lambda e: e.matmul(p[:], lhsT=onT[:, h, ts * 128:(ts + 1) * 128],
                                                      rhs=w_out[:, h, nh * 512:(nh + 1) * 512],
                                                      start=(h == 0), stop=(h == 7)),
                             reads=[b_onT[h], b_w], writes=[bp])
                    S.op("act", lambda e: e.activation(out=ymix[:, nh * 512:(nh + 1) * 512], in_=p[:], func=AF.Copy),
                         reads=[bp], writes=[b_ymix])
                epi.run(tile, ymix[:], [b_ymix], X_dst, bX_dst, XT_dst, bXT_dst)
        S.barrier()


TOPK = 256
IDX_SCALE = (8 ** -0.5) * (64 ** -0.5)
ATT_SCALE = 128 ** -0.5


def phase_dsa(S, nc, C, j, W, XT_src, bXT_src, X_res, bX_res, X_dst, bX_dst, XT_dst, bXT_dst,
              ln_g_row, ln_b_row, router, Gt, bG):
    with ExitStack() as st:
        sb = lambda name, shape, dt: st.enter_context(nc.sbuf_tensor(uq(name), shape, dt))
        ps = lambda name, shape, dt: st.enter_context(nc.psum_tensor(uq(name), shape, dt))
        win = sb("ds_win", [128, 8, 584], BF16)
        wuq = sb("ds_wuq", [128, 2, 1024], BF16)
        wqi = sb("ds_wqi", [128, 2, 512], BF16)
        wuk = sb("ds_wuk", [128, 8, 256], BF16)
        wuv = sb("ds_wuv", [128, 8, 2, 128], BF16)
        wout = sb("ds_wout", [128, 8, D], BF16)
        b_w = Buf("dsw")
        S.dma("pool", win[:], W["dsa_w_in"][j].rearrange("(kc p) n -> p kc n", p=128), writes=[b_w])
        S.dma("pool", wuq[:], W["dsa_w_uq"][j].rearrange("(kc p) n -> p kc n", p=128), writes=[b_w])
        S.dma("pool", wqi[:], W["dsa_w_qidx"][j].rearrange("(kc p) n -> p kc n", p=128), writes=[b_w])
        S.dma("pool", wuk[:], W["dsa_w_uk"][j].rearrange("h d c -> d h c"), writes=[b_w])
        S.dma("pool", wuv[:], W["dsa_w_uv"][j].rearrange("h (cc p) d -> p h cc d", p=128), writes=[b_w])
        S.dma("pool", wout[:], W["dsa_w_out"][j].rearrange("(kc p) n -> p kc n", p=128), writes=[b_w])
        gq = sb("ds_gq", [128, 256], F32)
        gkv = sb("ds_gkv", [128, 256], F32)
        kg = sb("ds_kg", [128, 64], F32)
        kb = sb("ds_kb", [128, 64], F32)
        b_g = Buf("dsg")
        S.dma("sp", gq[:], W["dsa_q_norm_g"][j].partition_broadcast(128), writes=[b_g])
        S.dma("sp", gkv[:], W["dsa_kv_norm_g"][j].partition_broadcast(128), writes=[b_g])
        S.dma("sp", kg[:], W["dsa_kidx_norm_g"][j].partition_broadcast(128), writes=[b_g])
        S.dma("sp", kb[:], W["dsa_kidx_norm_b"][j].partition_broadcast(128), writes=[b_g])
        onesb = sb("ds_ones", [128, 128], BF16)
        b_k = Buf("dsconst")
        S.op("pool", lambda e: e.memset(onesb[:], 1.0), writes=[b_k])
        ckvT = sb("ds_ckvT", [128, 2, SEQ], BF16)
        ckv = sb("ds_ckv", [128, NTS, 256], BF16)
        kidxT = sb("ds_kidxT", [64, SEQ], BF16)
        b_ckvT, b_ckv, b_kidxT = bufs(NTS, "ckvT"), bufs(NTS, "ckv"), bufs(NTS, "kidxT")
        xts = [sb("ds_xt%d" % i, [128, 8, 128], BF16) for i in range(2)]
        b_xts = bufs(2, "dsxt")
        cqT = sb("ds_cqT", [128, 2, 512], BF16)
        b_cqT = bufs(4, "cqT")
        qTh = [sb("ds_qTh%d" % i, [128, 512], BF16) for i in range(2)]
        b_qTh = bufs(2, "qTh")
        qlatT = sb("ds_qlatT", [128, 2, 8, 512], BF16)
        b_qlat = bufs(8, "qlat")
        qidxT = sb("ds_qidxT", [64, 8, 512], BF16)
        b_qidx = bufs(8, "qidx")
        widx = sb("ds_widx", [128, 4, 8], F32)
        b_widx = bufs(4, "widx")
        ss = sb("ds_ss", [128, 8], F32)
        b_ss = Buf("ss")
        junk = sb("ds_junk", [128, 256], BF16)
        b_junk = Buf("junk")
        cq = sb("ds_cq", [128, 256], BF16)
        b_cq = Buf("cq")
        kst = sb("ds_kst", [128, 16], F32)
        b_kst = Buf("kst")
        kx = sb("ds_kx", [128, 64], F32)
        kxb = sb("ds_kxb", [128, 64], BF16)
        b_kx = Buf("kx")
        acc = sb("ds_acc", [128, SEQ], F32)
        work = sb("ds_work", [128, SEQ], F32)
        b_acc, b_work = Buf("acc"), Buf("work")
        rr = [sb("ds_r%d" % i, [128, 512], F32) for i in range(2)]
        b_rr = bufs(2, "r")
        m8 = sb("ds_m8", [128, 8], F32)
        b_m8 = Buf("m8")
        masks = [sb("ds_mask%d" % i, [128, SEQ], BF16) for i in range(2)]
        b_masks = bufs(2, "mask")
        maskTs = [sb("ds_maskT%d" % i, [128, NTS, 128], BF16) for i in range(2)]
        b_maskTs = [bufs(NTS // 4, "maskTa"), bufs(NTS // 4, "maskTb")]
        E = [sb("ds_E%d" % i, [128, 4, 128], BF16) for i in range(2)]
        P = [sb("ds_P%d" % i, [128, 4, 128], BF16) for i in range(2)]
        b_E, b_P = bufs(2, "E"), bufs(2, "P")
        rden = sb("ds_rden", [128, 512], F32)
        b_rden = Buf("rden")
        posb = sb("ds_posb", [128, 4, 128], F32)
        b_posb = Buf("posb")
        olat = sb("ds_olat", [128, 2, 4, 128], BF16)
        b_olat = Buf("olat")
        oT = sb("ds_oT", [128, 8, 128], BF16)
        b_oT = bufs(2, "oT")
        pidx = [ps("ds_pidx%d" % i, [128, 512], F32) for i in range(2)]
        b_pidx = bufs(2, "pidx")
        pmt = ps("ds_pmt", [128, 8, 128], BF16)
        b_pmt = Buf("pmt")
        plg = [ps("ds_plg%d" % i, [128, 4, 128], F32) for i in range(2)]
        b_plg = bufs(2, "plg")
        polat = [ps("ds_pol%d" % i, [128, 4, 128], F32) for i in range(2)]
        b_pol = bufs(2, "pol")
        pden = ps("ds_pden", [128, 512], F32)
        b_pden = Buf("pden")
        epi = Epi(S, nc, st, C, ln_g_row, ln_b_row, router=router, pt=polat, b_pt=b_pol,
                  pr=pden[:, 0:NEXP], b_pr=b_pden, nbuf=1)

        for gb in range(T // 512):
            seq = gb // (SEQ // 512)
            blk = gb % (SEQ // 512)
            for ts in range(4):
                qi = blk * 4 + ts
                tcols = slice(ts * 128, (ts + 1) * 128)
                scols = slice(qi * 128, (qi + 1) * 128)
                xt, b_xt = xts[ts % 2], b_xts[ts % 2]
                S.dma("sp", xt[:], XT_src[gb * 4 + ts], reads=[bXT_src[gb * 4 + ts]], writes=[b_xt])
                pa0, bpa0 = plg[0], b_plg[0]
                pa1, bpa1 = plg[1], b_plg[1]
                pa0f = pa0[:].rearrange("p a b -> p (a b)")
                pa1f = pa1[:].rearrange("p a b -> p (a b)")
                for kc in range(8):
                    S.op("pe", lambda e: e.matmul(pa0f, lhsT=xt[:, kc, :], rhs=win[:, kc, 0:512],
                                                  start=(kc == 0), stop=(kc == 7)), reads=[b_xt, b_w], writes=[bpa0])
                for kc in range(8):
                    S.op("pe", lambda e: e.matmul(pa1f[:, 0:72], lhsT=xt[:, kc, :], rhs=win[:, kc, 512:584],
                                                  start=(kc == 0), stop=(kc == 7)), reads=[b_xt, b_w], writes=[bpa1])
                S.op("act", lambda e: e.activation(out=junk[:], in_=pa0f[:, 0:256], func=AF.Square,
                                                   accum_out=ss[:, 0:1]), reads=[bpa0], writes=[b_junk, b_ss])
                S.op("act", lambda e: e.activation(out=junk[:], in_=pa0f[:, 256:512], func=AF.Square,
                                                   accum_out=ss[:, 1:2]), reads=[bpa0], writes=[b_junk, b_ss])
                S.op("dve", lambda e: e.tensor_scalar(out=ss[:, 2:4], in0=ss[:, 0:2], scalar1=1.0 / 256.0,
                                                      scalar2=float(RMS_EPS), op0=ALU.mult, op1=ALU.add),
                     reads=[b_ss], writes=[b_ss])
                S.op("act", lambda e: e.activation(out=ss[:, 4:6], in_=ss[:, 2:4], func=AF.Ln), reads=[b_ss], writes=[b_ss])
                S.op("act", lambda e: e.activation(out=ss[:, 6:8], in_=ss[:, 4:6], func=AF.Exp, scale=-0.5),
                     reads=[b_ss], writes=[b_ss])
                S.op("dve", lambda e: e.scalar_tensor_tensor(out=cq[:], in0=pa0f[:, 0:256], scalar=ss[:, 6:7],
                                                             in1=gq[:], op0=ALU.mult, op1=ALU.mult),
                     reads=[bpa0, b_ss, b_g], writes=[b_cq])
                S.op("dve", lambda e: e.scalar_tensor_tensor(out=ckv[:, qi, :], in0=pa0f[:, 256:512], scalar=ss[:, 7:8],
                                                             in1=gkv[:], op0=ALU.mult, op1=ALU.mult),
                     reads=[bpa0, b_ss, b_g], writes=[b_ckv[qi]])
                S.op("dve", lambda e: e.bn_stats(out=kst[:, 0:6], in_=pa1f[:, 0:64]), reads=[bpa1], writes=[b_kst])
                S.op("dve", lambda e: e.bn_aggr(out=kst[:, 6:8], in_=kst[:, 0:6]), reads=[b_kst], writes=[b_kst])
                S.op("dve", lambda e: e.tensor_scalar_add(out=kst[:, 8:9], in0=kst[:, 7:8], scalar1=float(LN_EPS)),
                     reads=[b_kst], writes=[b_kst])
                S.op("act", lambda e: e.activation(out=kst[:, 9:10], in_=kst[:, 8:9], func=AF.Ln),
                     reads=[b_kst], writes=[b_kst])
                S.op("act", lambda e: e.activation(out=kst[:, 10:11], in_=kst[:, 9:10], func=AF.Exp, scale=-0.5),
                     reads=[b_kst], writes=[b_kst])
                S.op("dve", lambda e: e.tensor_scalar(out=kst[:, 11:12], in0=kst[:, 6:7], scalar1=-1.0,
                                                      scalar2=kst[:, 10:11], op0=ALU.mult, op1=ALU.mult),
                     reads=[b_kst], writes=[b_kst])
                S.op("act", lambda e: e.activation(out=kx[:], in_=pa1f[:, 0:64], func=AF.Identity,
                                                   scale=kst[:, 10:11], bias=kst[:, 11:12]),
                     reads=[bpa1, b_kst], writes=[b_kx])
                S.op("dve", lambda e: e.tensor_tensor(out=kx[:], in0=kx[:], in1=kg[:], op=ALU.mult),
                     reads=[b_kx, b_g], writes=[b_kx])
                S.op("dve", lambda e: e.tensor_tensor(out=kxb[:], in0=kx[:], in1=kb[:], op=ALU.add),
                     reads=[b_kx, b_g], writes=[b_kx])
                S.op("dve", lambda e: e.tensor_scalar_mul(out=widx[:, ts, :], in0=pa1f[:, 64:72], scalar1=float(IDX_SCALE)),
                     reads=[bpa1], writes=[b_widx[ts]])
                for cc in range(2):
                    S.op("pe", lambda e: e.transpose(pmt[:, cc, :], cq[:, cc * 128:(cc + 1) * 128], C.identb[:]),
                         reads=[b_cq, C.b_const], writes=[b_pmt])
                for cc in range(2):
                    S.op("pe", lambda e: e.transpose(pmt[:, 2 + cc, :], ckv[:, qi, cc * 128:(cc + 1) * 128], C.identb[:]),
                         reads=[b_ckv[qi], C.b_const], writes=[b_pmt])
                S.op("pe", lambda e: e.transpose(pmt[0:64, 4, :], kxb[:], C.identb[:]),
                     reads=[b_kx, C.b_const], writes=[b_pmt])
                S.op("act", lambda e: e.activation(out=cqT[:, :, tcols], in_=pmt[:, 0:2, :], func=AF.Copy),
                     reads=[b_pmt], writes=[b_cqT[ts]])
                S.op("act", lambda e: e.activation(out=ckvT[:, :, scols], in_=pmt[:, 2:4, :], func=AF.Copy),
                     reads=[b_pmt], writes=[b_ckvT[qi]])
                S.op("dve", lambda e: e.tensor_copy(out=kidxT[:, scols], in_=pmt[0:64, 4, :]),
                     reads=[b_pmt], writes=[b_kidxT[qi]])
            for h in range(8):
                pqh, bpqh = pidx[0], b_pidx[0]
                pqi, bpqi = pidx[1], b_pidx[1]
                qb, bqb = qTh[h % 2], b_qTh[h % 2]
                for cc in range(2):
                    S.op("pe", lambda e: e.matmul(pqh[:], lhsT=wuq[:, cc, h * 128:(h + 1) * 128], rhs=cqT[:, cc, :],
                                                  start=(cc == 0), stop=(cc == 1)), reads=[b_w] + b_cqT, writes=[bpqh])
                S.op("act", lambda e: e.activation(out=qb[:], in_=pqh[:], func=AF.Copy), reads=[bpqh], writes=[bqb])
                for cc in range(2):
                    pq_, bpq_ = polat[cc], b_pol[cc]
                    pq_f = pq_[:].rearrange("p a b -> p (a b)")
                    S.op("pe", lambda e: e.matmul(pq_f, lhsT=wuk[:, h, cc * 128:(cc + 1) * 128], rhs=qb[:],
                                                  start=True, stop=True), reads=[b_w, bqb], writes=[bpq_])
                    if cc == 0:
                        S.op("dve", lambda e: e.tensor_copy(out=qlatT[:, cc, h, :], in_=pq_f), reads=[bpq_],
                             writes=[b_qlat[h]])
                    else:
                        S.op("act", lambda e: e.activation(out=qlatT[:, cc, h, :], in_=pq_f, func=AF.Copy),
                             reads=[bpq_], writes=[b_qlat[h]])
                for cc in range(2):
                    S.op("pe", lambda e: e.matmul(pqi[0:64, :], lhsT=wqi[:, cc, h * 64:(h + 1) * 64], rhs=cqT[:, cc, :],
                                                  start=(cc == 0), stop=(cc == 1)), reads=[b_w] + b_cqT, writes=[bpqi])
                S.op("dve", lambda e: e.tensor_copy(out=qidxT[:, h, :], in_=pqi[0:64, :]), reads=[bpqi],
                     writes=[b_qidx[h]])
            def part_b1(ts):
                qi = blk * 4 + ts
                tile = gb * 4 + ts
                tcols = slice(ts * 128, (ts + 1) * 128)
                L = (qi + 1) * 128
                maskT, b_maskT = maskTs[ts % 2], b_maskTs[ts % 2]
                mask, b_mask = masks[ts % 2], b_masks[ts % 2]
                nseg = (L + 511) // 512
                for sg_ in range(nseg):
                    wd_ = min(512, L - sg_ * 512)
                    kcols = slice(sg_ * 512, sg_ * 512 + wd_)
                    kread = b_kidxT[sg_ * 4:sg_ * 4 + (wd_ // 128)]
                    for h in range(8):
                        k2 = (sg_ * 8 + h) % 2
                        S.op("pe", lambda e: e.matmul(pidx[k2][:, 0:wd_], lhsT=qidxT[:, h, tcols], rhs=kidxT[:, kcols],
                                                      start=True, stop=True),
                             reads=[b_qidx[h]] + kread, writes=[b_pidx[k2]])
                        S.op("act", lambda e: e.activation(out=rr[k2][:, 0:wd_], in_=pidx[k2][:, 0:wd_], func=AF.Relu),
                             reads=[b_pidx[k2]], writes=[b_rr[k2]])
                        if h == 0:
                            S.op("dve", lambda e: e.tensor_scalar_mul(out=acc[:, kcols], in0=rr[k2][:, 0:wd_],
                                                                      scalar1=widx[:, ts, 0:1]),
                                 reads=[b_rr[k2], b_widx[ts]], writes=[b_acc])
                        else:
                            S.op("dve", lambda e: e.scalar_tensor_tensor(out=acc[:, kcols], in0=rr[k2][:, 0:wd_],
                                                                         scalar=widx[:, ts, h:h + 1], in1=acc[:, kcols],
                                                                         op0=ALU.mult, op1=ALU.add),
                                 reads=[b_rr[k2], b_widx[ts], b_acc], writes=[b_acc])
                S.op("pool", lambda e: e.affine_select(out=acc[:, qi * 128:L], in_=acc[:, qi * 128:L], pattern=[[-1, 128]],
                                                       compare_op=ALU.is_ge, fill=C.negbig_reg, base=0,
                                                       channel_multiplier=1), reads=[b_acc], writes=[b_acc])
                if qi >= 2:
                    nr = TOPK // 8
                    for r in range(nr):
                        src = acc if r == 0 else work
                        bsrc = b_acc if r == 0 else b_work
                        S.op("dve", lambda e: e.max(out=m8[:], in_=src[:, 0:L]), reads=[bsrc], writes=[b_m8])
                        if r < nr - 1:
                            S.op("dve", lambda e: e.match_replace(out=work[:, 0:L], in_to_replace=m8[:],
                                                                  in_values=src[:, 0:L], imm_value=float(NEG_BIG)),
                                 reads=[bsrc, b_m8], writes=[b_work])
                    S.op("dve", lambda e: e.tensor_scalar(out=mask[:, 0:L], in0=acc[:, 0:L], scalar1=m8[:, 7:8],
                                                          scalar2=None, op0=ALU.is_ge), reads=[b_acc, b_m8], writes=[b_mask])
                else:
                    S.op("dve", lambda e: e.tensor_scalar(out=mask[:, 0:L], in0=acc[:, 0:L], scalar1=-1e29,
                                                          scalar2=None, op0=ALU.is_ge), reads=[b_acc], writes=[b_mask])

            def part_b2(ts):
                qi = blk * 4 + ts
                tile = gb * 4 + ts
                tcols = slice(ts * 128, (ts + 1) * 128)
                L = (qi + 1) * 128
                maskT, b_maskT = maskTs[ts % 2], b_maskTs[ts % 2]
                mask, b_mask = masks[ts % 2], b_masks[ts % 2]
                epi.prefetch(X_res, bX_res, tile)
                for g4 in range((qi + 4) // 4):
                    n4 = min(4, qi + 1 - g4 * 4)
                    for q4 in range(n4):
                        st_ = g4 * 4 + q4
                        S.op("pe", lambda e: e.transpose(pmt[:, q4, :], mask[:, st_ * 128:(st_ + 1) * 128], C.identb[:]),
                             reads=[b_mask, C.b_const], writes=[b_pmt])
                    S.op("act", lambda e: e.activation(out=maskT[:, g4 * 4:g4 * 4 + n4, :], in_=pmt[:, 0:n4, :],
                                                       func=AF.Copy), reads=[b_pmt], writes=[b_maskT[g4]])
                for hg in range(2):
                    for st_ in range(qi + 1):
                        k2 = st_ % 2
                        plf = plg[k2][:].rearrange("p a b -> p (a b)")
                        for cc in range(2):
                            S.op("pe", lambda e: e.matmul(plg[k2][:], lhsT=ckvT[:, cc, st_ * 128:(st_ + 1) * 128],
                                                          rhs=qlatT[:, cc, hg * 4:(hg + 1) * 4, tcols],
                                                          start=(cc == 0), stop=(cc == 1)),
                                 reads=[b_ckvT[st_]] + b_qlat[hg * 4:(hg + 1) * 4], writes=[b_plg[k2]])
                        S.op("act", lambda e: e.activation(out=E[k2][:], in_=plg[k2][:], func=AF.Exp, scale=float(ATT_SCALE)),
                             reads=[b_plg[k2]], writes=[b_E[k2]])
                        S.op("pool", lambda e: e.tensor_tensor(out=P[k2][:], in0=E[k2][:],
                                                               in1=maskT[:, st_, :].unsqueeze(1).broadcast_to([128, 4, 128]),
                                                               op=ALU.mult),
                             reads=[b_E[k2], b_maskT[st_ // 4]], writes=[b_P[k2]])
                        for cc in range(2):
                            S.op("pe", lambda e: e.matmul(polat[cc][:], lhsT=ckv[:, st_, cc * 128:(cc + 1) * 128], rhs=P[k2][:],
                                                          start=(st_ == 0), stop=(st_ == qi)),
                                 reads=[b_ckv[st_], b_P[k2]], writes=[b_pol[cc]])
                        S.op("pe", lambda e: e.matmul(pden[:], lhsT=onesb[:], rhs=P[k2][:].rearrange("p a b -> p (a b)"),
                                                      start=(st_ == 0), stop=(st_ == qi)),
                             reads=[b_k, b_P[k2]], writes=[b_pden])
                    S.op("act", lambda e: e.activation(out=rden[:], in_=pden[:], func=AF.Ln), reads=[b_pden], writes=[b_rden])
                    S.op("act", lambda e: e.activation(out=rden[:], in_=rden[:], func=AF.Exp, scale=-1.0),
                         reads=[b_rden], writes=[b_rden])
                    for cc in range(2):
                        S.op("act", lambda e: e.activation(out=olat[:, cc, :, :], in_=polat[cc][:], func=AF.Copy),
                             reads=[b_pol[cc]], writes=[b_olat])
                    po_, bpo_ = plg[0], b_plg[0]
                    for h4 in range(4):
                        h = hg * 4 + h4
                        for cc in range(2):
                            S.op("pe", lambda e: e.matmul(po_[:, h4, :], lhsT=wuv[:, h, cc, :], rhs=olat[:, cc, h4, :],
                                                          start=(cc == 0), stop=(cc == 1)),
                                 reads=[b_w, b_olat], writes=[bpo_])
                    S.op("act", lambda e: e.activation(out=posb[:], in_=po_[:], func=AF.Copy), reads=[bpo_], writes=[b_posb])
                    S.op("pool", lambda e: e.tensor_tensor(out=oT[:, hg * 4:(hg + 1) * 4, :], in0=posb[:],
                                                           in1=rden[:].rearrange("p (a b) -> p a b", a=4), op=ALU.mult),
                         reads=[b_posb, b_rden], writes=[b_oT[hg]])
                for nh in range(2):
                    for h in range(8):
                        S.op("pe", lambda e: e.matmul(pidx[nh][:], lhsT=oT[:, h, :], rhs=wout[:, h, nh * 512:(nh + 1) * 512],
                                                      start=(h == 0), stop=(h == 7)),
                             reads=[b_oT[h // 4], b_w], writes=[b_pidx[nh]])
                epi.run(tile, [pidx[0][:], pidx[1][:]], [b_pidx[0], b_pidx[1]], X_dst, bX_dst, XT_dst, bXT_dst,
                        gates_dst=Gt, bG=bG)

            part_b1(0)
            for ts in range(4):
                if ts + 1 < 4:
                    part_b1(ts + 1)
                part_b2(ts)
        S.barrier()


def emit_consts(S, nc, st, C):
    C.ident = st.enter_context(nc.sbuf_tensor("c_ident", [128, 128], F32))
    C.identb = st.enter_context(nc.sbuf_tensor("c_identb", [128, 128], BF16))
    C.b_const = Buf("const")
    C.negbig_reg = nc.gpsimd.to_reg(float(NEG_BIG))
    S.op("pool", lambda e: e.memset(C.ident[:], 0.0), writes=[C.b_const])
    S.op("pool", lambda e: e.affine_select(out=C.ident[:], in_=C.ident[:], pattern=[[-1, 128]],
                                           compare_op=ALU.not_equal, fill=1.0, base=0, channel_multiplier=1),
         reads=[C.b_const], writes=[C.b_const])
    S.op("pool", lambda e: e.tensor_copy(out=C.identb[:], in_=C.ident[:]), reads=[C.b_const], writes=[C.b_const])


WEIGHT_SPECS = [
    ("ln_g", [4, 2, 1024]), ("ln_b", [4, 2, 1024]),
    ("hg_w_in", [2, 1024, 4096]), ("hg_lower_bounds", [2, 1024]), ("hg_norm_g", [2, 8, 128]),
    ("hg_w_out", [2, 1024, 1024]),
    ("dsa_w_in", [2, 1024, 584]), ("dsa_q_norm_g", [2, 256]), ("dsa_kv_norm_g", [2, 256]),
    ("dsa_w_uq", [2, 256, 1024]), ("dsa_w_uk", [2, 8, 128, 256]), ("dsa_w_uv", [2, 8, 256, 128]),
    ("dsa_w_qidx", [2, 256, 512]), ("dsa_kidx_norm_g", [2, 64]), ("dsa_kidx_norm_b", [2, 64]),
    ("dsa_w_out", [2, 1024, 1024]),
    ("ffn_w_gate_up", [2, NFC, 128, 2 * 8 * FC]), ("ffn_w_down", [2, NFC, 128, (FC // 128) * 1024]),
    ("moe_w_router", [2, 128, 8 * 8]), ("moe_w_gate_up", [2, 8, NFC, 128, 2 * 8 * FC]),
    ("moe_w_down", [2, 8, NFC, 128, (FC // 128) * 1024]),
]


def build_program(plan=None):
    if plan is None:
        plan = list(range(8))
    nc = bass.Bass("TRN2", target_bir_lowering=False)
    x_in = nc.dram_tensor("x", [T, D], F32, kind="ExternalInput").ap()
    W = {name: nc.dram_tensor(name, shape, F32, kind="ExternalInput").ap() for name, shape in WEIGHT_SPECS}
    out = nc.dram_tensor("out", [T, D], F32, kind="ExternalOutput").ap()
    Xs = nc.dram_tensor("Xs", [T, D], F32, kind="Internal").ap()
    XT = [nc.dram_tensor("XT%d" % i, [NT, 128, 8, 128], BF16, kind="Internal").ap() for i in range(2)]
    Gt = nc.dram_tensor("Gt", [T, NEXP], F32, kind="Internal").ap()
    bX_in = bufs(NT, "xin")
    bXs = bufs(NT, "Xs")
    bXT = [bufs(NT, "XT0_"), bufs(NT, "XT1_")]
    bG = bufs(NT, "G")
    bOut = bufs(NT, "out")
    C = Ctx()
    with ExitStack() as st:
        S = Sched(nc, st)
        emit_consts(S, nc, st, C)
        cur_X, cur_bX = x_in, bX_in
        cur = 0
        phase_prep(S, nc, C, x_in, bX_in, XT[0], bXT[0])
        for si, sub in enumerate(plan):
            layer, kind = sub // 2, sub % 2
            j = layer // 2
            last = si == len(plan) - 1
            X_dst, bX_dst = (out, bOut) if last else (Xs, bXs)
            XT_dst, bXT_dst = (None, None) if last else (XT[1 - cur], bXT[1 - cur])
            if kind == 1:
                if layer % 2 == 0:
                    experts = [(W["ffn_w_gate_up"][j], W["ffn_w_down"][j])]
                    gates = None
                else:
                    experts = [(W["moe_w_gate_up"][j, e], W["moe_w_down"][j, e]) for e in range(NEXP)]
                    gates = Gt
                phase_ffn(S, nc, C, XT[cur], bXT[cur], cur_X, cur_bX, X_dst, bX_dst, XT_dst, bXT_dst,
                          experts, gates, bG, W["ln_g"][layer, 1], W["ln_b"][layer, 1])
            elif layer % 2 == 0:
                phase_hgrn(S, nc, C, j, W, XT[cur], bXT[cur], cur_X, cur_bX, X_dst, bX_dst, XT_dst, bXT_dst,
                           W["ln_g"][layer, 0], W["ln_b"][layer, 0])
            else:
                phase_dsa(S, nc, C, j, W, XT[cur], bXT[cur], cur_X, cur_bX, X_dst, bX_dst, XT_dst, bXT_dst,
                          W["ln_g"][layer, 0], W["ln_b"][layer, 0], W["moe_w_router"][j], Gt, bG)
            cur_X, cur_bX = X_dst, bX_dst
            cur = 1 - cur
        S.barrier()
        C.ninst = S.ninst
    return nc


def relayout_weights(inputs):
    out = {}
    for name, _ in WEIGHT_SPECS:
        w = np.asarray(inputs[name], dtype=np.float32)
        if name in ("ffn_w_gate_up", "moe_w_gate_up"):
            lead = w.shape[:-2]
            w = w.reshape(lead + (8, 128, 2, NFC, FC))
            nl = len(lead)
            perm = tuple(range(nl)) + (nl + 3, nl + 1, nl + 2, nl + 0, nl + 4)
            w = w.transpose(perm).reshape(lead + (NFC, 128, 2 * 8 * FC))
        elif name == "moe_w_router":
            w = w.reshape(2, 8, 128, NEXP).transpose(0, 2, 1, 3).reshape(2, 128, 8 * NEXP)
        elif name in ("ffn_w_down", "moe_w_down"):
            lead = w.shape[:-2]
            w = w.reshape(lead + (NFC, FC // 128, 128, D))
            nl = len(lead)
            perm = tuple(range(nl)) + (nl + 0, nl + 2, nl + 1, nl + 3)
            w = w.transpose(perm).reshape(lead + (NFC, 128, (FC // 128) * D))
        out[name] = np.ascontiguousarray(w)
    return out


_PROG = {}


def kernel(**inputs):
    x = np.ascontiguousarray(inputs["x"], dtype=np.float32)
    if "full" not in _PROG:
        _PROG["full"] = build_program()
    nc = _PROG["full"]
    wmap = relayout_weights(inputs)
    in_maps = []
    for c in range(8):
        m = dict(wmap)
        m["x"] = x[c * NSEQ:(c + 1) * NSEQ].reshape(T, D)
        in_maps.append(m)
    res = run_bass_kernel_spmd(nc, in_maps, core_ids=list(range(8)))
    outs = [np.asarray(r["out"]).reshape(NSEQ, SEQ, D) for r in res.results]
    return np.concatenate(outs, axis=0).astype(np.float32)
```
